# Optimizing a Trainium2 kernel written in Bass

```python
import jax
import jax.numpy as jnp
from jax import lax
import numpy as np

D_MODEL = 1024
BATCH = 2
SEQ = 8192
DEPTH = 1

N_MEM = 256
EPS = 1e-6

GLA_HEADS = 4
GLA_DK = D_MODEL // 16
GLA_DV = D_MODEL // 8
GLA_RANK = 16
GLA_GATE_NORMALIZER = 16.0
GLA_CHUNK = 64

MOBA_HEADS = 4
MOBA_DH = D_MODEL // 16
MOBA_BLOCK = 256
MOBA_TOPK = 3
MOBA_QBLOCK = 128

MEM_HEADS = 4
MEM_DH = D_MODEL // 16

GLA_QK_W = GLA_HEADS * GLA_DK
GLA_V_W = GLA_HEADS * GLA_DV
MOBA_W = MOBA_HEADS * MOBA_DH
MEM_W = MEM_HEADS * MEM_DH
D_MIX = GLA_V_W + MOBA_W + MEM_W
IN_SPLITS = (GLA_QK_W, GLA_QK_W, GLA_V_W, GLA_V_W, GLA_RANK, MOBA_W, MOBA_W, MOBA_W, MEM_W)
D_IN = sum(IN_SPLITS)

N_GROUPS = 4
EXPERTS_PER_GROUP = 8
N_EXPERTS = N_GROUPS * EXPERTS_PER_GROUP
MOE_TOPK = 2
MOE_FF = D_MODEL // 2
MOE_BLOCK = 128

kernel_name = 'hymba_gla_moba_mem_hmoe_layer'


def rmsnorm(x, g):
    xf = x.astype(jnp.float32)
    y = xf * lax.rsqrt(jnp.mean(xf * xf, axis=-1, keepdims=True) + EPS)
    return (y * g.astype(jnp.float32)).astype(x.dtype)


def split_heads(t, n):
    b, s, w = t.shape
    return t.reshape(b, s, n, w // n).transpose(0, 2, 1, 3)


def merge_heads(t):
    b, n, s, d = t.shape
    return t.transpose(0, 2, 1, 3).reshape(b, s, n * d)


def alibi_slopes(n):
    return jnp.asarray(2.0 ** (-8.0 * np.arange(1, n + 1) / n), dtype=jnp.float32)


def gla_chunked(q, k, v, g):
    f32 = jnp.float32
    b, h, s, dk = q.shape
    dv = v.shape[-1]
    c = GLA_CHUNK
    n = s // c
    qf = q.astype(f32).reshape(b, h, n, c, dk) * (dk ** -0.5)
    kf = k.astype(f32).reshape(b, h, n, c, dk)
    vf = v.astype(f32).reshape(b, h, n, c, dv)
    cum = jnp.cumsum(g.astype(f32).reshape(b, h, n, c, dk), axis=3)
    cum_last = cum[:, :, :, -1:, :]
    q_dec = qf * jnp.exp(cum)
    k_inv = kf * jnp.exp(-cum)
    causal = jnp.tril(jnp.ones((c, c), dtype=bool))
    a_intra = jnp.where(causal, jnp.einsum('bhnik,bhnjk->bhnij', q_dec, k_inv), 0.0)
    o_intra = jnp.einsum('bhnij,bhnjv->bhniv', a_intra, vf)
    k_end = kf * jnp.exp(cum_last - cum)
    d_state = jnp.einsum('bhnck,bhncv->nbhkv', k_end, vf)
    chunk_decay = jnp.moveaxis(jnp.exp(cum_last[:, :, :, 0, :]), 2, 0)

    def step(state, inp):
        dec, ds = inp
        return state * dec[..., None] + ds, state

    _, s_prev = lax.scan(step, jnp.zeros((b, h, dk, dv), f32), (chunk_decay, d_state))
    o_inter = jnp.einsum('bhnck,nbhkv->bhncv', q_dec, s_prev)
    return (o_intra + o_inter).reshape(b, h, s, dv)


def moba_attention(q, k, v, slopes):
    f32 = jnp.float32
    b, h, s, d = q.shape
    nb = -(-s // MOBA_BLOCK)
    pad = nb * MOBA_BLOCK - s
    k_p = jnp.pad(k, ((0, 0), (0, 0), (0, pad), (0, 0)))
    v_p = jnp.pad(v, ((0, 0), (0, 0), (0, pad), (0, 0)))
    kb = k_p.reshape(b, h, nb, MOBA_BLOCK, d)
    vb = v_p.reshape(b, h, nb, MOBA_BLOCK, d)
    k_mean = jnp.mean(kb.astype(f32), axis=3)
    nb_gate = max(nb, MOBA_TOPK)
    scale = d ** -0.5
    bi = jnp.arange(b)[:, None, None, None]
    hi = jnp.arange(h)[None, :, None, None]
    blk_pos = jnp.arange(MOBA_BLOCK)
    m = slopes[None, :, None, None]
    n_sel = MOBA_TOPK * MOBA_BLOCK

    def query_block(qb):
        start = qb * MOBA_QBLOCK
        qc = lax.dynamic_slice_in_dim(q, start, MOBA_QBLOCK, axis=2)
        t = start + jnp.arange(MOBA_QBLOCK)
        own = start // MOBA_BLOCK
        gate = jnp.einsum('bhqd,bhnd->bhqn', qc.astype(f32), k_mean)
        gate = jnp.where(jnp.arange(nb) < own, gate, -jnp.inf)
        gate = jnp.pad(gate, ((0, 0), (0, 0), (0, 0), (0, nb_gate - nb)), constant_values=-jnp.inf)
        _, idx = lax.top_k(gate, MOBA_TOPK)
        idx = jnp.minimum(idx, nb - 1)
        valid = jnp.arange(MOBA_TOPK) < own
        k_sel = kb[bi, hi, idx]
        v_sel = vb[bi, hi, idx]
        s_sel = jnp.einsum('bhqd,bhqkjd->bhqkj', qc, k_sel, preferred_element_type=f32) * scale
        dist_sel = (t[:, None, None] - (idx[..., None] * MOBA_BLOCK + blk_pos)).astype(f32)
        s_sel = jnp.where(valid[:, None], s_sel - m[..., None] * dist_sel, -jnp.inf)
        k_own = lax.dynamic_slice_in_dim(k_p, own * MOBA_BLOCK, MOBA_BLOCK, axis=2)
        v_own = lax.dynamic_slice_in_dim(v_p, own * MOBA_BLOCK, MOBA_BLOCK, axis=2)
        dist_own = (t[:, None] - (own * MOBA_BLOCK + blk_pos)[None, :]).astype(f32)
        s_own = jnp.einsum('bhqd,bhjd->bhqj', qc, k_own, preferred_element_type=f32) * scale
        s_own = jnp.where(dist_own >= 0, s_own - m * dist_own, -jnp.inf)
        scores = jnp.concatenate([s_sel.reshape(b, h, MOBA_QBLOCK, n_sel), s_own], axis=-1)
        p = jax.nn.softmax(scores, axis=-1)
        p_sel = p[..., :n_sel].reshape(b, h, MOBA_QBLOCK, MOBA_TOPK, MOBA_BLOCK).astype(v.dtype)
        p_own = p[..., n_sel:].astype(v.dtype)
        o = (jnp.einsum('bhqkj,bhqkjd->bhqd', p_sel, v_sel, preferred_element_type=f32)
             + jnp.einsum('bhqj,bhjd->bhqd', p_own, v_own, preferred_element_type=f32))
        return o.astype(q.dtype)

    out = lax.map(query_block, jnp.arange(s // MOBA_QBLOCK))
    return out.transpose(1, 2, 0, 3, 4).reshape(b, h, s, d)


def memory_attention(q, mk, mv):
    f32 = jnp.float32
    scale = q.shape[-1] ** -0.5
    s = jnp.einsum('bhsd,bhmd->bhsm', q, mk, preferred_element_type=f32) * scale
    p = jax.nn.softmax(s, axis=-1).astype(mv.dtype)
    return jnp.einsum('bhsm,bhmd->bhsd', p, mv, preferred_element_type=f32).astype(q.dtype)


def hierarchical_moe(h, w_rg, b_rg, w_re, b_re, w_gate, w_up, w_down):
    f32 = jnp.float32
    b, s, d = h.shape
    t_tok = b * s
    xt = h.reshape(t_tok, d)
    lg = jnp.einsum('td,dg->tg', xt, w_rg).astype(f32) + b_rg.astype(f32)
    g_sel = jnp.argmax(lg, axis=-1)
    p_group = jnp.take_along_axis(jax.nn.softmax(lg, axis=-1), g_sel[:, None], axis=-1)
    le = jnp.einsum('td,gde->tge', xt, w_re).astype(f32) + b_re.astype(f32)
    le = jnp.take_along_axis(le, g_sel[:, None, None], axis=1)[:, 0]
    le_top, e_loc = lax.top_k(le, MOE_TOPK)
    gates = p_group * jax.nn.softmax(le_top, axis=-1)
    expert = (g_sel[:, None] * EXPERTS_PER_GROUP + e_loc).astype(jnp.int32)

    n_assign = t_tok * MOE_TOPK
    e_flat = expert.reshape(n_assign)
    t_flat = jnp.repeat(jnp.arange(t_tok, dtype=jnp.int32), MOE_TOPK)
    w_flat = gates.reshape(n_assign)
    order = jnp.argsort(e_flat)
    e_s, t_s, w_s = e_flat[order], t_flat[order], w_flat[order]
    counts = jax.ops.segment_sum(jnp.ones((n_assign,), jnp.int32), e_flat, num_segments=N_EXPERTS)
    padded = (counts + MOE_BLOCK - 1) // MOE_BLOCK * MOE_BLOCK
    starts = jnp.cumsum(counts) - counts
    pstarts = jnp.cumsum(padded) - padded
    dest = pstarts[e_s] + (jnp.arange(n_assign, dtype=jnp.int32) - starts[e_s])
    n_blocks = (n_assign + N_EXPERTS * (MOE_BLOCK - 1) + MOE_BLOCK - 1) // MOE_BLOCK
    n_rows = n_blocks * MOE_BLOCK
    tok_buf = jnp.full((n_rows,), t_tok, jnp.int32).at[dest].set(t_s)
    w_buf = jnp.zeros((n_rows,), f32).at[dest].set(w_s)
    block_start = jnp.arange(n_blocks, dtype=jnp.int32) * MOE_BLOCK
    block_expert = jnp.minimum(jnp.searchsorted(pstarts + padded, block_start, side='right'), N_EXPERTS - 1)
    x_pad = jnp.concatenate([xt, jnp.zeros((1, d), xt.dtype)], axis=0)

    def expert_block(args):
        toks, e = args
        xb = x_pad[toks]
        hid = jax.nn.silu(xb @ w_gate[e]) * (xb @ w_up[e])
        return hid @ w_down[e]

    y = lax.map(expert_block, (tok_buf.reshape(n_blocks, MOE_BLOCK), block_expert))
    y = y.reshape(n_rows, d) * w_buf[:, None].astype(y.dtype)
    out = jnp.zeros((t_tok + 1, d), y.dtype).at[tok_buf].add(y)[:t_tok]
    return out.reshape(b, s, d)


def setup_inputs(seed: int = 0) -> dict:
    key = jax.random.key(seed)
    ks = jax.random.split(key, 24)
    L = DEPTH

    def nrm(k, shape, fan_in):
        return jax.random.normal(k, shape, jnp.float32) * fan_in ** -0.5

    def gain(k, shape):
        return 1.0 + 0.02 * jax.random.normal(k, shape, jnp.float32)

    return {
        'x': jax.random.normal(ks[0], (BATCH, SEQ, D_MODEL), jnp.float32),
        'mem': jax.random.normal(ks[1], (BATCH, N_MEM, D_MODEL), jnp.float32),
        'attn_norm_g': gain(ks[2], (L, D_MODEL)),
        'mem_norm_g': gain(ks[3], (L, D_MODEL)),
        'w_in': nrm(ks[4], (L, D_MODEL, D_IN), D_MODEL),
        'w_gla_gk': nrm(ks[5], (L, GLA_RANK, GLA_QK_W), GLA_RANK),
        'b_gla_gk': 0.1 * jax.random.normal(ks[6], (L, GLA_QK_W), jnp.float32),
        'gla_out_norm_g': gain(ks[7], (L, GLA_DV)),
        'moba_q_norm_g': gain(ks[8], (L, MOBA_DH)),
        'moba_k_norm_g': gain(ks[9], (L, MOBA_DH)),
        'w_mem_kv': nrm(ks[10], (L, D_MODEL, 2 * MEM_W), D_MODEL),
        'mem_q_norm_g': gain(ks[11], (L, MEM_DH)),
        'mem_k_norm_g': gain(ks[12], (L, MEM_DH)),
        'w_out': nrm(ks[13], (L, D_MIX, D_MODEL), D_MIX),
        'ffn_norm_g': gain(ks[14], (L, D_MODEL)),
        'w_router_group': nrm(ks[15], (L, D_MODEL, N_GROUPS), D_MODEL),
        'b_router_group': 0.01 * jax.random.normal(ks[16], (L, N_GROUPS), jnp.float32),
        'w_router_expert': nrm(ks[17], (L, N_GROUPS, D_MODEL, EXPERTS_PER_GROUP), D_MODEL),
        'b_router_expert': 0.01 * jax.random.normal(ks[18], (L, N_GROUPS, EXPERTS_PER_GROUP), jnp.float32),
        'w_gate': nrm(ks[19], (L, N_EXPERTS, D_MODEL, MOE_FF), D_MODEL),
        'w_up': nrm(ks[20], (L, N_EXPERTS, D_MODEL, MOE_FF), D_MODEL),
        'w_down': nrm(ks[21], (L, N_EXPERTS, MOE_FF, D_MODEL), MOE_FF),
    }


def reference(x, mem, attn_norm_g, mem_norm_g, w_in, w_gla_gk, b_gla_gk, gla_out_norm_g,
              moba_q_norm_g, moba_k_norm_g, w_mem_kv, mem_q_norm_g, mem_k_norm_g, w_out,
              ffn_norm_g, w_router_group, b_router_group, w_router_expert, b_router_expert,
              w_gate, w_up, w_down):
    slopes = alibi_slopes(MOBA_HEADS)
    split_at = np.cumsum(IN_SPLITS)[:-1].tolist()
    for l in range(DEPTH):
        h = rmsnorm(x, attn_norm_g[l])
        proj = jnp.einsum('bsd,de->bse', h, w_in[l])
        gla_q, gla_k, gla_v, gla_r, gla_lr, mb_q, mb_k, mb_v, mem_q = jnp.split(proj, split_at, axis=-1)

        g_log = jax.nn.log_sigmoid((gla_lr @ w_gla_gk[l] + b_gla_gk[l]).astype(jnp.float32)) / GLA_GATE_NORMALIZER
        o_gla = gla_chunked(split_heads(gla_q, GLA_HEADS), split_heads(gla_k, GLA_HEADS),
                            split_heads(gla_v, GLA_HEADS), split_heads(g_log, GLA_HEADS))
        o_gla = rmsnorm(o_gla, gla_out_norm_g[l]).astype(x.dtype)
        o_gla = merge_heads(o_gla) * jax.nn.silu(gla_r)

        q_b = rmsnorm(split_heads(mb_q, MOBA_HEADS), moba_q_norm_g[l])
        k_b = rmsnorm(split_heads(mb_k, MOBA_HEADS), moba_k_norm_g[l])
        v_b = split_heads(mb_v, MOBA_HEADS)
        o_moba = merge_heads(moba_attention(q_b, k_b, v_b, slopes))

        mem_h = rmsnorm(mem, mem_norm_g[l])
        mem_k, mem_v = jnp.split(jnp.einsum('bmd,de->bme', mem_h, w_mem_kv[l]), 2, axis=-1)
        q_c = rmsnorm(split_heads(mem_q, MEM_HEADS), mem_q_norm_g[l])
        k_c = rmsnorm(split_heads(mem_k, MEM_HEADS), mem_k_norm_g[l])
        o_mem = merge_heads(memory_attention(q_c, k_c, split_heads(mem_v, MEM_HEADS)))

        mix = jnp.concatenate([o_gla, o_moba, o_mem], axis=-1)
        x = x + jnp.einsum('bse,ed->bsd', mix, w_out[l])

        x = x + hierarchical_moe(rmsnorm(x, ffn_norm_g[l]), w_router_group[l], b_router_group[l],
                                 w_router_expert[l], b_router_expert[l], w_gate[l], w_up[l], w_down[l])
    return x
```

```python
import contextlib
import numpy as np
import concourse.bass as bass
import concourse.mybir as mybir
from concourse.bass_utils import run_bass_kernel_spmd

F32 = mybir.dt.float32
BF16 = mybir.dt.bfloat16
AF = mybir.ActivationFunctionType
ALU = mybir.AluOpType
AX = mybir.AxisListType

SEQ = 8192
DM = 1024
NT = SEQ // 128
NSB = SEQ // 512
EPS = 1e-6
BIG = 30000.0
NEXP = 32
FF = 512
TOK2 = 2048


class FW:
    ENG = ("pe", "act", "dve", "pool", "sp")
    EMAP = {"pe": "tensor", "act": "scalar", "dve": "vector", "pool": "gpsimd", "sp": "sync"}

    def __init__(self, nc, tag="", n_dma_sems=20):
        self.nc = nc
        self.ops = {e: [] for e in self.ENG}
        self.sem = {e: nc.alloc_semaphore("c%s_%s" % (tag, e)) for e in self.ENG}
        self.cnt = {e: 0 for e in self.ENG}
        nd = {"sp": 6, "pool": 4}
        self.dsems = {q: [nc.alloc_semaphore("d%s%s_%d" % (tag, q, i)) for i in range(n)] for q, n in nd.items()}
        self.dcnt = {q: [0] * n for q, n in nd.items()}
        self.dnext = {q: 0 for q in nd}
        self.known = {e: {} for e in self.ENG}
        self.lastw = {}
        self.readers = {}
        self.tag = tag
        self.ncoll = 0
        self.limit = None
        self.nops = 0
        self.log = []

    @staticmethod
    def _is_psum(k):
        if isinstance(k, tuple):
            k = k[0]
        return isinstance(k, str) and (k in ("PO", "PS") or (len(k) == 2 and k[0] == "B" and k[1].isdigit()))

    def _skip(self, eng, reads, writes):
        self.nops += 1
        if self.limit is not None and self.nops > self.limit:
            return True
        self.log.append((self.nops, eng, tuple(reads), tuple(writes)))
        return False

    def _deps(self, eng, reads, writes):
        deps = []
        for k in reads:
            if k in self.lastw:
                deps.append(self.lastw[k])
        for k in writes:
            if k in self.lastw:
                deps.append(self.lastw[k])
            deps.extend(self.readers.get(k, []))
        waits = {}
        for (sem, val, deng) in deps:
            if deng == eng and eng == "pe":
                continue
            kn = self.known[eng].get(sem.name, 0)
            if val <= kn:
                continue
            if sem.name not in waits or waits[sem.name][1] < val:
                waits[sem.name] = (sem, val)
        for name, (sem, val) in waits.items():
            self.known[eng][name] = val
        return list(waits.values())

    def _record(self, reads, writes, tok):
        for k in writes:
            self.lastw[k] = tok
            self.readers[k] = []
        for k in reads:
            self.readers.setdefault(k, []).append(tok)

    def op(self, eng, fn, reads=(), writes=()):
        if self._skip(eng, reads, writes):
            return
        ex = [k for k in reads if self._is_psum(k) and k not in writes]
        if ex:
            writes = list(writes) + ex
        waits = self._deps(eng, reads, writes)
        self.cnt[eng] += 1
        tok = (self.sem[eng], self.cnt[eng], eng)
        self.ops[eng].append((waits, fn, self.sem[eng], 1))
        self._record(reads, writes, tok)

    def dma(self, eng, fn, reads=(), writes=()):
        if self._skip(eng, reads, writes):
            return
        waits = self._deps(eng, reads, writes)
        i = self.dnext[eng]
        self.dnext[eng] = (i + 1) % len(self.dsems[eng])
        sem = self.dsems[eng][i]
        prev = self.dcnt[eng][i]
        if prev > 0 and self.known[eng].get(sem.name, 0) < prev:
            waits.append((sem, prev))
            self.known[eng][sem.name] = prev
        self.dcnt[eng][i] += 16
        tok = (sem, self.dcnt[eng][i], "dma")
        self.ops[eng].append((waits, fn, sem, 16))
        self._record(reads, writes, tok)

    def coll(self, fn, reads=(), writes=()):
        waits = self._deps("pool", reads, writes)
        sem = self.nc.alloc_semaphore("cc%s_%d" % (self.tag, self.ncoll))
        self.ncoll += 1
        tok = (sem, 1, "coll")
        self.ops["pool"].append((waits, fn, sem, 1))
        self._record(reads, writes, tok)

    def final_wait(self, eng):
        keys = list(self.lastw.keys())
        waits = self._deps(eng, keys, keys)
        self.ops[eng].append((waits, None, None, 0))

    def emit(self):
        nc = self.nc
        for e in ("sp", "pool", "act"):
            self.final_wait(e)
        with nc.Block() as block:
            for e in self.ENG:
                ops = self.ops[e]

                def body(engine, ops=ops):
                    for (waits, fn, sem, inc) in ops:
                        for (ws, wv) in waits:
                            engine.wait_ge(ws, wv)
                        if fn is not None:
                            ins = fn(engine)
                            ins.then_inc(sem, inc)
                getattr(block, self.EMAP[e])(body)
        n = {e: len(self.ops[e]) for e in self.ENG}
        self.ops = {e: [] for e in self.ENG}
        return n


def build_program(debug=0, nsb=NSB, n_exp=NEXP):
    nc = bass.Bass("TRN2", target_bir_lowering=False)

    def din(name, shape, dt=F32):
        return nc.dram_tensor(name, list(shape), dt, kind="ExternalInput").ap()

    xb = din("xb", [SEQ, DM])
    xo = din("xo", [TOK2, DM])
    memb = din("memb", [256, DM])
    w_in_ab = din("w_in_ab", [DM, 640])
    w_in_lrT = din("w_in_lrT", [16, DM])
    w_gk = din("w_gk", [16, 64])
    b_gk = din("b_gk", [1, 64])
    g_attn = din("g_attn", [128, 8])
    g_mem = din("g_mem", [128, 8])
    g_ffn = din("g_ffn", [128, 8])
    g_gla = din("g_gla", [128, 1])
    g_qk = din("g_qk", [1, 256])
    w_memkv = din("w_memkv", [DM, 128])
    w_out_p = din("w_out_p", [DM, DM])
    w_r = din("w_r", [DM, 36])
    b_r = din("b_r", [1, 36])
    if debug != 2:
        w_gate = din("w_gate", [NEXP, 128, 8, FF])
        w_up = din("w_up", [NEXP, 128, 8, FF])
        w_down = din("w_down", [NEXP, 128, 4, DM])
    kaug_c = din("kaug_c", [34, SEQ])
    qaug_c = din("qaug_c", [2, 512])
    cbt_d = din("cbt", [1, 1024])
    cm_d = din("cm", [128, 2048])
    ident_d = din("ident", [128, 128])
    triu_d = din("triu", [128, 128])
    out = nc.dram_tensor("out", [TOK2, DM], F32, kind="ExternalOutput").ap()
    dbg = None
    if debug:
        dbg = nc.dram_tensor("dbg_mix", [4, 1024, 2048], BF16, kind="ExternalOutput").ap()
        dbg2 = nc.dram_tensor("dbg_gin", [4, 256, 2048], BF16, kind="ExternalOutput").ap()

    gin = nc.dram_tensor("gin", [4, 256, 2048], BF16)
    gall = nc.dram_tensor("gall", [4, 1024, 2048], BF16)

    fw = FW(nc, "a")
    import os
    if os.environ.get("FW_LIMIT"):
        fw.limit = int(os.environ["FW_LIMIT"])
    es = contextlib.ExitStack()
    with es:
        def S(name, shape, dt):
            return es.enter_context(nc.sbuf_tensor("s1_" + name, list(shape), dt))

        def P(name, shape, dt):
            return es.enter_context(nc.psum_tensor("p1_" + name, list(shape), dt))

        idf = S("idf", [128, 128], F32)
        idb = S("idb", [128, 128], BF16)
        triu = S("triu", [128, 128], F32)
        ones32 = S("ones32", [128, 128], F32)
        onesb = S("onesb", [128, 128], BF16)
        w_in = S("w_in", [128, 8, 704], BF16)
        lrT = S("lrT", [16, DM], F32)
        wgk = S("wgk", [16, 64], F32)
        bgk = S("bgk", [128, 64], F32)
        gat = S("gat", [128, 8], F32)
        gme = S("gme", [128, 8], F32)
        ggla = S("ggla", [128, 1], F32)
        gqk = S("gqk", [128, 256], F32)
        wmkv = S("wmkv", [128, 8, 128], BF16)
        KT = S("KT", [128, SEQ], BF16)
        VA = S("VA", [128, NT, 128], BF16)
        QT = [S("QT%d" % i, [128, 512], BF16) for i in range(2)]
        CQT = [S("CQT%d" % i, [64, 512], BF16) for i in range(2)]
        cbt = S("cbt", [128, 1024], F32)
        cm = S("cm", [128, 2048], BF16)
        MKT = S("MKT", [64, 256], BF16)
        MVA = S("MVA", [128, 2, 128], BF16)
        gatebuf = S("gatebuf", [128, 32], F32)
        ksum = S("ksum", [64, NT], F32)
        kmT = S("kmT", [64, 32], F32)
        Sst = S("Sst", [64, 128], F32)
        Sbf = S("Sbf", [64, 128], BF16)
        xs = [S("xs%d" % i, [128, DM], F32) for i in range(2)]
        junk = S("junk", [128, DM], BF16)
        ss = S("ss", [128, 4], F32)
        rs = S("rs", [128, 4], F32)
        xn = S("xn", [128, DM], BF16)
        pj1s = [S("pj1s%d" % i, [128, 512], F32) for i in range(2)]
        pj2s = [S("pj2s%d" % i, [128, 192], F32) for i in range(2)]
        hT = S("hT", [128, 8, 128], BF16)
        ssq = S("ssq", [128, 4], F32)
        rs3 = S("rs3", [128, 4], F32)
        qn32 = S("qn32", [128, 64], F32)
        kn32 = S("kn32", [128, 64], F32)
        cqn = S("cqn", [128, 64], BF16)
        QT32 = S("QT32", [64, 128], F32)
        max8 = S("max8", [128, 8], F32)
        selpen = S("selpen", [128, 32], F32)
        qs = S("qs", [128, 96], BF16)
        zz = S("zz", [128, 64], F32)
        e1 = S("e1", [128, 64], F32)
        ll = S("ll", [128, 64], F32)
        ecum = S("ecum", [128, 64], F32)
        einv = S("einv", [128, 64], F32)
        eL = S("eL", [128, 64], F32)
        decay = S("decay", [64, 1], F32)
        kinv32 = S("kinv32", [128, 64], F32)
        qk2 = S("qk2", [128, 128], BF16)
        kend = S("kend", [128, 64], BF16)
        qkT = S("qkT", [64, 256], BF16)
        ATm = S("ATm", [128, 128], BF16)
        vg = S("vg", [128, 128], BF16)
        sq = S("sq", [128, 128], BF16)
        lnt = S("lnt", [128, 128], F32)
        rstdb = S("rstdb", [128, 128], F32)
        e2 = S("e2", [128, 128], F32)
        sr = S("sr", [128, 128], BF16)
        t1 = S("t1", [128, 128], F32)
        ogT = [S("ogT%d" % i, [128, 512], BF16) for i in range(2)]
        omT = [S("omT%d" % i, [64, 512], BF16) for i in range(2)]
        ocT = [S("ocT%d" % i, [64, 512], BF16) for i in range(2)]
        pexp = [S("pexp%d" % i, [128, 512], BF16) for i in range(3)]
        rden = S("rden", [64, 512], F32)
        memx = S("memx", [128, DM], F32)
        B0 = P("B0", [128, 1024], BF16)
        B1 = P("B1", [128, 512], F32)
        B2 = P("B2", [128, 512], F32)
        B3 = P("B3", [128, 512], F32)
        B4 = P("B4", [128, 512], F32)
        PS = [P("PS0", [128, 512], F32), P("PS1", [128, 512], F32)]
        PO = P("PO", [128, 512], F32)
        B4h = B2[:, 192:512].bitcast(BF16)

        fw.dma("sp", lambda e: e.dma_start(out=idf[:], in_=ident_d), writes=["idf"])
        fw.dma("pool", lambda e: e.dma_start(out=idb[:], in_=ident_d), writes=["idb"])
        fw.dma("sp", lambda e: e.dma_start(out=triu[:], in_=triu_d), writes=["triu"])
        fw.op("dve", lambda e: e.memset(ones32[:], 1.0), writes=["ones32"])
        fw.op("dve", lambda e: e.memset(onesb[:], 1.0), writes=["onesb"])
        fw.dma("pool", lambda e: e.dma_start(out=w_in[:, :, 0:640], in_=w_in_ab.rearrange("(c p) n -> p c n", p=128)), writes=["w_in"])
        fw.dma("sp", lambda e: e.dma_start(out=lrT[:], in_=w_in_lrT), writes=["lrT"])
        fw.dma("sp", lambda e: e.dma_start(out=wgk[:], in_=w_gk), writes=["wgk"])
        fw.dma("sp", lambda e: e.dma_start(out=bgk[:], in_=b_gk.partition_broadcast(128)), writes=["bgk"])
        fw.dma("sp", lambda e: e.dma_start(out=gat[:], in_=g_attn), writes=["gat"])
        fw.dma("sp", lambda e: e.dma_start(out=gme[:], in_=g_mem), writes=["gme"])
        fw.dma("sp", lambda e: e.dma_start(out=ggla[:], in_=g_gla), writes=["ggla"])
        fw.dma("sp", lambda e: e.dma_start(out=gqk[:], in_=g_qk.partition_broadcast(128)), writes=["gqk"])
        fw.dma("pool", lambda e: e.dma_start(out=wmkv[:], in_=w_memkv.rearrange("(c p) n -> p c n", p=128)), writes=["wmkv"])
        fw.dma("pool", lambda e: e.dma_start(out=KT[64:98, :], in_=kaug_c), writes=["KTc"])
        for i in range(2):
            fw.dma("pool", lambda e, i=i: e.dma_start(out=QT[i][96:98, :], in_=qaug_c), writes=[("QT", i)])
        fw.dma("sp", lambda e: e.dma_start(out=cbt[:], in_=cbt_d.partition_broadcast(128)), writes=["cbt"])
        fw.dma("pool", lambda e: e.dma_start(out=cm[:], in_=cm_d), writes=["cm"])
        fw.op("dve", lambda e: e.memset(VA[:, :, 64:128], 1.0), writes=["VAc"])
        fw.op("dve", lambda e: e.memset(MVA[:, :, 64:128], 1.0), writes=["MVA"])
        fw.op("dve", lambda e: e.memset(gatebuf[:], -1e30), writes=["gatebuf"])
        fw.op("dve", lambda e: e.memset(Sst[:], 0.0), writes=["Sst"])
        fw.op("dve", lambda e: e.memset(Sbf[:], 0.0), writes=["Sbf"])
        fw.op("dve", lambda e: e.memset(selpen[:], 0.0), writes=["selpen"])
        fw.op("dve", lambda e: e.tensor_scalar(out=gqk[:, 0:64], in0=gqk[:, 0:64], scalar1=0.125, scalar2=None, op0=ALU.mult), reads=["gqk"], writes=["gqk"])
        fw.op("dve", lambda e: e.tensor_scalar(out=gqk[:, 128:192], in0=gqk[:, 128:192], scalar1=0.125, scalar2=None, op0=ALU.mult), reads=["gqk"], writes=["gqk"])
        for c in range(8):
            fw.op("pe", lambda e, c=c: e.matmul(B1[:, c * 64:(c + 1) * 64], lhsT=lrT[:, c * 128:(c + 1) * 128], rhs=wgk[:], start=True, stop=True),
                  reads=["lrT", "wgk"], writes=["B1"])
        fw.op("dve", lambda e: e.tensor_copy(out=w_in[:, :, 640:704], in_=B1[:, 0:512].rearrange("p (c k) -> p c k", c=8)), reads=["B1"], writes=["w_in"])

        def rms_rows(src_key, src_ap, n, dst_rs):
            fw.op("act", lambda e: e.activation(out=junk[:, 0:n], in_=src_ap, func=AF.Square, accum_out=ss[:, 0:1]), reads=[src_key], writes=["junk", "ss"])
            fw.op("act", lambda e: e.activation(out=rs[:, 0:1], in_=ss[:, 0:1], func=AF.Ln, scale=1.0 / n, bias=EPS), reads=["ss"], writes=["rs"])
            fw.op("act", lambda e: e.activation(out=dst_rs, in_=rs[:, 0:1], func=AF.Exp, scale=-0.5), reads=["rs"], writes=["rs1"])

        def norm_and_transpose(x_key, x_ap, gains):
            rms_rows(x_key, x_ap, DM, rs[:, 1:2])
            fw.op("dve", lambda e: e.tensor_scalar(out=xn[:], in0=x_ap, scalar1=rs[:, 1:2], scalar2=None, op0=ALU.mult), reads=[x_key, "rs1"], writes=["xn"])
            for c in range(8):
                fw.op("pe", lambda e, c=c: e.transpose(out=B0[:, c * 128:(c + 1) * 128], in_=xn[:, c * 128:(c + 1) * 128], identity=idb[:]),
                      reads=["xn", "idb"], writes=["B0"])
            fw.op("dve", lambda e: e.tensor_tensor(out=hT[:], in0=B0[:, :].rearrange("p (c t) -> p c t", c=8),
                                                   in1=gains[:, :].unsqueeze(2).to_broadcast([128, 8, 128]), op=ALU.mult),
                  reads=["B0", "gat", "gme"], writes=["hT"])

        for mt in range(2):
            fw.dma("sp", lambda e, mt=mt: e.dma_start(out=memx[:], in_=memb[mt * 128:(mt + 1) * 128, :]), writes=["memx"])
            norm_and_transpose("memx", memx[:], gme)
            for c in range(8):
                fw.op("pe", lambda e, c=c: e.matmul(B2[:, 0:128], lhsT=hT[:, c, :], rhs=wmkv[:, c, :], start=(c == 0), stop=(c == 7)),
                      reads=["hT", "wmkv"], writes=["B2"])
            fw.op("act", lambda e: e.activation(out=junk[:, 0:64], in_=B2[:, 0:64], func=AF.Square, accum_out=ssq[:, 0:1]), reads=["B2"], writes=["junk", "ssq"])
            fw.op("act", lambda e: e.activation(out=rs3[:, 0:1], in_=ssq[:, 0:1], func=AF.Ln, scale=1.0 / 64, bias=EPS), reads=["ssq"], writes=["rs3"])
            fw.op("act", lambda e: e.activation(out=rs3[:, 0:1], in_=rs3[:, 0:1], func=AF.Exp, scale=-0.5), reads=["rs3"], writes=["rs3"])
            fw.op("dve", lambda e: e.scalar_tensor_tensor(out=cqn[:], in0=B2[:, 0:64], scalar=rs3[:, 0:1], in1=gqk[:, 192:256], op0=ALU.mult, op1=ALU.mult),
                  reads=["B2", "rs3", "gqk"], writes=["cqn"])
            fw.op("pe", lambda e: e.transpose(out=B4h[0:64, 0:128], in_=cqn[:], identity=idb[:]), reads=["cqn", "idb"], writes=["B2"])
            fw.op("dve", lambda e, mt=mt: e.tensor_copy(out=MKT[:, mt * 128:(mt + 1) * 128], in_=B4h[0:64, 0:128]), reads=["B2"], writes=["MKT"])
            fw.op("act", lambda e, mt=mt: e.copy(out=MVA[:, mt, 0:64], in_=B2[:, 64:128]), reads=["B2"], writes=["MVA"])

        def tile_head(tt):
            xi = tt % 2
            x_key, x_ap = ("xs", xi), xs[xi][:]
            fw.dma("sp", lambda e: e.dma_start(out=xs[xi][:], in_=xb[tt * 128:(tt + 1) * 128, :]), writes=[("xs", xi)])
            yield
            rms_rows(x_key, x_ap, DM, rs[:, 1:2])
            yield
            fw.op("dve", lambda e: e.tensor_scalar(out=xn[:], in0=x_ap, scalar1=rs[:, 1:2], scalar2=None, op0=ALU.mult), reads=[x_key, "rs1"], writes=["xn"])
            yield
            for c in range(8):
                fw.op("pe", lambda e, c=c: e.transpose(out=B0[:, c * 128:(c + 1) * 128], in_=xn[:, c * 128:(c + 1) * 128], identity=idb[:]),
                      reads=["xn", "idb"], writes=["B0"])
                if c % 4 == 3:
                    yield
            fw.op("dve", lambda e: e.tensor_tensor(out=hT[:], in0=B0[:, :].rearrange("p (c t) -> p c t", c=8),
                                                   in1=gat[:, :].unsqueeze(2).to_broadcast([128, 8, 128]), op=ALU.mult),
                  reads=["B0", "gat", "gme"], writes=["hT"])
            yield
            for c in range(8):
                fw.op("pe", lambda e, c=c: e.matmul(B1[:, 0:512], lhsT=hT[:, c, :], rhs=w_in[:, c, 0:512], start=(c == 0), stop=(c == 7)),
                      reads=["hT", "w_in"], writes=["B1"])
            for c in range(8):
                fw.op("pe", lambda e, c=c: e.matmul(B2[:, 0:192], lhsT=hT[:, c, :], rhs=w_in[:, c, 512:704], start=(c == 0), stop=(c == 7)),
                      reads=["hT", "w_in"], writes=["B2"])
            yield
            fw.op("act", lambda e: e.copy(out=pj1s[xi][:], in_=B1[:, 0:512]), reads=["B1"], writes=[("pj1", xi)])
            fw.op("dve", lambda e: e.tensor_copy(out=pj2s[xi][:], in_=B2[:, 0:192]), reads=["B2"], writes=[("pj2", xi)])
            yield

        def tile_step(tt, nxt=None):
            s, a = tt // 4, tt % 4
            sb = s % 2
            xi = tt % 2
            own = tt // 2
            fw.op("act", lambda e: e.activation(out=junk[:, 0:64], in_=pj1s[xi][:, 384:448], func=AF.Square, accum_out=ssq[:, 0:1]), reads=[("pj1", xi)], writes=["junk", "ssq"])
            fw.op("act", lambda e: e.activation(out=junk[:, 0:64], in_=pj1s[xi][:, 448:512], func=AF.Square, accum_out=ssq[:, 1:2]), reads=[("pj1", xi)], writes=["junk", "ssq"])
            fw.op("act", lambda e: e.activation(out=junk[:, 0:64], in_=pj2s[xi][:, 64:128], func=AF.Square, accum_out=ssq[:, 2:3]), reads=[("pj2", xi)], writes=["junk", "ssq"])
            fw.op("act", lambda e: e.activation(out=rs3[:, 0:3], in_=ssq[:, 0:3], func=AF.Ln, scale=1.0 / 64, bias=EPS), reads=["ssq"], writes=["rs3"])
            fw.op("act", lambda e: e.activation(out=rs3[:, 0:3], in_=rs3[:, 0:3], func=AF.Exp, scale=-0.5), reads=["rs3"], writes=["rs3"])
            fw.op("dve", lambda e: e.scalar_tensor_tensor(out=qn32[:], in0=pj1s[xi][:, 384:448], scalar=rs3[:, 0:1], in1=gqk[:, 0:64], op0=ALU.mult, op1=ALU.mult),
                  reads=[("pj1", xi), "rs3", "gqk"], writes=["qn32"])
            fw.op("dve", lambda e: e.scalar_tensor_tensor(out=kn32[:], in0=pj1s[xi][:, 448:512], scalar=rs3[:, 1:2], in1=gqk[:, 64:128], op0=ALU.mult, op1=ALU.mult),
                  reads=[("pj1", xi), "rs3", "gqk"], writes=["kn32"])
            fw.op("dve", lambda e: e.scalar_tensor_tensor(out=cqn[:], in0=pj2s[xi][:, 64:128], scalar=rs3[:, 2:3], in1=gqk[:, 128:192], op0=ALU.mult, op1=ALU.mult),
                  reads=[("pj2", xi), "rs3", "gqk"], writes=["cqn"])
            def chain_a():
                yield
                fw.op("pe", lambda e: e.transpose(out=B3[0:64, 0:128], in_=kn32[:], identity=idf[:]), reads=["kn32", "idf"], writes=["B3"])
                fw.op("dve", lambda e: e.tensor_copy(out=KT[0:64, tt * 128:(tt + 1) * 128], in_=B3[0:64, 0:128]), reads=["B3"], writes=[("KT", tt)])
                fw.op("dve", lambda e: e.reduce_sum(out=ksum[:, tt:tt + 1], in_=B3[0:64, 0:128], axis=AX.X), reads=["B3"], writes=["ksum"])
                if tt % 2 == 1:
                    n = tt // 2
                    fw.op("dve", lambda e: e.tensor_tensor(out=kmT[:, n:n + 1], in0=ksum[:, tt - 1:tt], in1=ksum[:, tt:tt + 1], op=ALU.add), reads=["ksum"], writes=["kmT"])
                    fw.op("dve", lambda e: e.tensor_scalar(out=kmT[:, n:n + 1], in0=kmT[:, n:n + 1], scalar1=1.0 / 256, scalar2=None, op0=ALU.mult), reads=["kmT"], writes=["kmT"])
                fw.op("act", lambda e: e.copy(out=VA[:, tt, 0:64], in_=pj2s[xi][:, 0:64]), reads=[("pj2", xi)], writes=[("VA", tt)])
                yield
                fw.op("act", lambda e: e.copy(out=qs[:, 0:64], in_=qn32[:]), reads=["qn32"], writes=["qs"])
                if own > 0:
                    fw.op("pe", lambda e: e.transpose(out=B3[0:64, 128:256], in_=qn32[:], identity=idf[:]), reads=["qn32", "idf"], writes=["B3"])
                    fw.op("dve", lambda e: e.tensor_copy(out=QT32[:], in_=B3[0:64, 128:256]), reads=["B3"], writes=["QT32"])
                    yield
                    fw.op("pe", lambda e: e.matmul(B3[:, 256:256 + own], lhsT=QT32[:], rhs=kmT[:, 0:own], start=True, stop=True), reads=["QT32", "kmT"], writes=["B3"])
                    fw.op("dve", lambda e: e.tensor_copy(out=gatebuf[:, 0:own], in_=B3[:, 256:256 + own]), reads=["B3"], writes=["gatebuf"])
                    fw.op("dve", lambda e: e.max(out=max8[:], in_=gatebuf[:]), reads=["gatebuf"], writes=["max8"])
                    fw.op("dve", lambda e: e.tensor_scalar(out=selpen[:, 0:own], in0=gatebuf[:, 0:own], scalar1=max8[:, 2:3], scalar2=None, op0=ALU.is_ge),
                          reads=["gatebuf", "max8"], writes=["selpen"])
                    fw.op("dve", lambda e: e.scalar_tensor_tensor(out=qs[:, 64:64 + own], in0=selpen[:, 0:own], scalar=BIG, in1=cbt[:, own * 32:own * 32 + own],
                                                                  op0=ALU.mult, op1=ALU.add), reads=["selpen", "cbt"], writes=["qs"])
                fw.op("dve", lambda e: e.tensor_copy(out=qs[:, 64 + own:96], in_=cbt[:, own * 32 + own:own * 32 + 32]), reads=["cbt"], writes=["qs"])
                yield
                fw.op("pe", lambda e: e.transpose(out=B4h[0:96, 0:128], in_=qs[:], identity=idb[:]), reads=["qs", "idb"], writes=["B2"])
                fw.op("dve", lambda e: e.tensor_copy(out=QT[sb][0:96, a * 128:(a + 1) * 128], in_=B4h[0:96, 0:128]), reads=["B2"], writes=[("QT", sb)])
                yield
                fw.op("pe", lambda e: e.transpose(out=B4h[0:64, 128:256], in_=cqn[:], identity=idb[:]), reads=["cqn", "idb"], writes=["B2"])
                fw.op("dve", lambda e: e.tensor_copy(out=CQT[sb][:, a * 128:(a + 1) * 128], in_=B4h[0:64, 128:256]), reads=["B2"], writes=[("CQT", sb)])
                yield

            def chain_b():
                yield
                fw.op("dve", lambda e: e.tensor_tensor(out=zz[:], in0=pj2s[xi][:, 128:192], in1=bgk[:], op=ALU.add), reads=[("pj2", xi), "bgk"], writes=["zz"])
                fw.op("act", lambda e: e.activation(out=e1[:], in_=zz[:], func=AF.Exp, scale=-1.0), reads=["zz"], writes=["e1"])
                fw.op("act", lambda e: e.activation(out=ll[:], in_=e1[:], func=AF.Ln, bias=1.0), reads=["e1"], writes=["ll"])
                fw.op("pe", lambda e: e.matmul(B4[:, 0:64], lhsT=triu[:], rhs=ll[:], start=True, stop=True), reads=["triu", "ll"], writes=["B4"])
                fw.op("pe", lambda e: e.matmul(B4[:, 64:128], lhsT=ones32[:], rhs=ll[:], start=True, stop=True), reads=["ones32", "ll"], writes=["B4"])
                fw.op("pe", lambda e: e.matmul(B4[0:64, 128:129], lhsT=ll[:], rhs=ones32[:, 0:1], start=True, stop=True), reads=["ones32", "ll"], writes=["B4"])
                yield
                fw.op("act", lambda e: e.activation(out=ecum[:], in_=B4[:, 0:64], func=AF.Exp, scale=-1.0 / 16), reads=["B4"], writes=["ecum"])
                fw.op("act", lambda e: e.activation(out=einv[:], in_=B4[:, 0:64], func=AF.Exp, scale=1.0 / 16), reads=["B4"], writes=["einv"])
                fw.op("act", lambda e: e.activation(out=eL[:], in_=B4[:, 64:128], func=AF.Exp, scale=-1.0 / 16), reads=["B4"], writes=["eL"])
                fw.op("act", lambda e: e.activation(out=decay[:], in_=B4[0:64, 128:129], func=AF.Exp, scale=-1.0 / 16), reads=["B4"], writes=["decay"])
                fw.op("dve", lambda e: e.scalar_tensor_tensor(out=qk2[:, 0:64], in0=pj1s[xi][:, 0:64], scalar=0.125, in1=ecum[:], op0=ALU.mult, op1=ALU.mult),
                      reads=[("pj1", xi), "ecum"], writes=["qk2"])
                fw.op("dve", lambda e: e.tensor_tensor(out=kinv32[:], in0=pj1s[xi][:, 64:128], in1=einv[:], op=ALU.mult), reads=[("pj1", xi), "einv"], writes=["kinv32"])
                fw.op("dve", lambda e: e.tensor_copy(out=qk2[:, 64:128], in_=kinv32[:]), reads=["kinv32"], writes=["qk2"])
                fw.op("dve", lambda e: e.tensor_tensor(out=kend[:], in0=kinv32[:], in1=eL[:], op=ALU.mult), reads=["kinv32", "eL"], writes=["kend"])
                yield
                fw.op("act", lambda e: e.copy(out=vg[:], in_=pj1s[xi][:, 128:256]), reads=[("pj1", xi)], writes=["vg"])
                fw.op("pe", lambda e: e.transpose(out=B4h[0:64, 256:384], in_=qk2[:, 0:64], identity=idb[:]), reads=["qk2", "idb"], writes=["B2"])
                fw.op("pe", lambda e: e.transpose(out=B4h[0:64, 384:512], in_=qk2[:, 64:128], identity=idb[:]), reads=["qk2", "idb"], writes=["B2"])
                fw.op("dve", lambda e: e.tensor_copy(out=qkT[:], in_=B4h[0:64, 256:512]), reads=["B2"], writes=["qkT"])
                yield
                fw.op("pe", lambda e: e.matmul(B3[:, 0:128], lhsT=qkT[:, 128:256], rhs=qkT[:, 0:128], start=True, stop=True), reads=["qkT"], writes=["B3"])
                fw.op("dve", lambda e: e.tensor_tensor(out=ATm[:], in0=B3[:, 0:128], in1=triu[:], op=ALU.mult), reads=["B3", "triu"], writes=["ATm"])
                fw.op("pe", lambda e: e.matmul(B4[:, 256:384], lhsT=vg[:], rhs=ATm[:], start=True, stop=False), reads=["vg", "ATm"], writes=["B4"])
                fw.op("pe", lambda e: e.matmul(B4[:, 256:384], lhsT=Sbf[:], rhs=qkT[:, 0:128], start=False, stop=True), reads=["Sbf", "qkT"], writes=["B4"])
                yield
                fw.op("pe", lambda e: e.matmul(B3[0:64, 256:384], lhsT=kend[:], rhs=vg[:], start=True, stop=True), reads=["kend", "vg"], writes=["B3"])
                fw.op("dve", lambda e: e.scalar_tensor_tensor(out=Sst[:], in0=Sst[:], scalar=decay[:, 0:1], in1=B3[0:64, 256:384], op0=ALU.mult, op1=ALU.add),
                      reads=["Sst", "decay", "B3"], writes=["Sst"])
                fw.op("act", lambda e: e.copy(out=Sbf[:], in_=Sst[:]), reads=["Sst"], writes=["Sbf"])
                yield
                fw.op("act", lambda e: e.activation(out=sq[:], in_=B4[:, 256:384], func=AF.Square), reads=["B4"], writes=["sq"])
                fw.op("pe", lambda e: e.matmul(B3[:, 0:128], lhsT=onesb[:], rhs=sq[:], start=True, stop=True), reads=["onesb", "sq"], writes=["B3"])
                fw.op("act", lambda e: e.activation(out=lnt[:], in_=B3[:, 0:128], func=AF.Ln, scale=1.0 / 128, bias=EPS), reads=["B3"], writes=["lnt"])
                fw.op("act", lambda e: e.activation(out=rstdb[:], in_=lnt[:], func=AF.Exp, scale=-0.5), reads=["lnt"], writes=["rstdb"])
                yield
                fw.op("act", lambda e: e.activation(out=e2[:], in_=pj1s[xi][:, 256:384], func=AF.Exp, scale=-1.0), reads=[("pj1", xi)], writes=["e2"])
                fw.op("dve", lambda e: e.tensor_scalar(out=e2[:], in0=e2[:], scalar1=1.0, scalar2=None, op0=ALU.add), reads=["e2"], writes=["e2"])
                fw.op("dve", lambda e: e.reciprocal(out=e2[:], in_=e2[:]), reads=["e2"], writes=["e2"])
                fw.op("dve", lambda e: e.tensor_tensor(out=sr[:], in0=pj1s[xi][:, 256:384], in1=e2[:], op=ALU.mult), reads=[("pj1", xi), "e2"], writes=["sr"])
                yield
                fw.op("pe", lambda e: e.transpose(out=B4h[:, 512:640], in_=sr[:], identity=idb[:]), reads=["sr", "idb"], writes=["B2"])
                fw.op("dve", lambda e: e.tensor_tensor(out=t1[:], in0=B4[:, 256:384], in1=rstdb[:], op=ALU.mult), reads=["B4", "rstdb"], writes=["t1"])
                fw.op("dve", lambda e: e.scalar_tensor_tensor(out=ogT[sb][:, a * 128:(a + 1) * 128], in0=t1[:], scalar=ggla[:, 0:1], in1=B4h[:, 512:640],
                                                              op0=ALU.mult, op1=ALU.mult), reads=["t1", "ggla", "B2"], writes=[("ogT", sb)])
                yield

            gens_ = [chain_a(), chain_b()] + ([nxt] if nxt is not None else [])
            while gens_:
                for g_ in list(gens_):
                    try:
                        next(g_)
                    except StopIteration:
                        gens_.remove(g_)
                yield

        def attn_block(s):
            sb = s % 2
            q4, s4 = s // 4, s % 4
            nkt = 4 * s + 4

            def qk(kt):
                pb = kt % 2
                diag = kt >= 4 * s
                fw.op("pe", lambda e: e.matmul(PS[pb][:, :], lhsT=KT[0:98, kt * 128:(kt + 1) * 128], rhs=QT[sb][0:98, :], start=True, stop=not diag),
                      reads=["KTc", ("KT", kt), ("QT", sb)], writes=[("PS", pb)])
                if diag:
                    c = kt - 4 * s
                    fw.op("pe", lambda e: e.matmul(PS[pb][:, :], lhsT=idb[:], rhs=cm[:, c * 512:(c + 1) * 512], start=False, stop=True),
                          reads=["idb", "cm"], writes=[("PS", pb)])
            qk(0)
            for kt in range(nkt):
                if kt + 1 < nkt:
                    qk(kt + 1)
                pb, xb_ = kt % 2, kt % 3
                fw.op("act", lambda e, pb=pb, xb_=xb_: e.activation(out=pexp[xb_][:], in_=PS[pb][:, :], func=AF.Exp), reads=[("PS", pb)], writes=[("pexp", xb_)])
                fw.op("pe", lambda e, kt=kt, xb_=xb_: e.matmul(PO[:, :], lhsT=VA[:, kt, :], rhs=pexp[xb_][:], start=(kt == 0), stop=(kt == nkt - 1)),
                      reads=["VAc", ("VA", kt), ("pexp", xb_)], writes=["PO"])
                yield
            fw.op("dve", lambda e: e.reciprocal(out=rden[:], in_=PO[64:128, :]), reads=["PO"], writes=["rden"])
            fw.op("dve", lambda e: e.tensor_tensor(out=omT[sb][:], in0=PO[0:64, :], in1=rden[:], op=ALU.mult), reads=["PO", "rden"], writes=[("omT", sb)])
            yield
            for mt in range(2):
                fw.op("pe", lambda e, mt=mt: e.matmul(PS[mt][:, :], lhsT=MKT[:, mt * 128:(mt + 1) * 128], rhs=CQT[sb][:, :], start=True, stop=True),
                      reads=["MKT", ("CQT", sb)], writes=[("PS", mt)])
            for mt in range(2):
                fw.op("act", lambda e, mt=mt: e.activation(out=pexp[mt][:], in_=PS[mt][:, :], func=AF.Exp), reads=[("PS", mt)], writes=[("pexp", mt)])
                fw.op("pe", lambda e, mt=mt: e.matmul(PO[:, :], lhsT=MVA[:, mt, :], rhs=pexp[mt][:], start=(mt == 0), stop=(mt == 1)),
                      reads=["MVA", ("pexp", mt)], writes=["PO"])
            fw.op("dve", lambda e: e.reciprocal(out=rden[:], in_=PO[64:128, :]), reads=["PO"], writes=["rden"])
            fw.op("dve", lambda e: e.tensor_tensor(out=ocT[sb][:], in0=PO[0:64, :], in1=rden[:], op=ALU.mult), reads=["PO", "rden"], writes=[("ocT", sb)])
            yield
            cs = slice(s4 * 512, (s4 + 1) * 512)
            g = gin.ap()
            fw.dma("pool", lambda e: e.dma_start(out=g[q4, 0:128, cs], in_=ogT[sb][:]), reads=[("ogT", sb)], writes=[("gin", q4, s4, 0)])
            fw.dma("pool", lambda e: e.dma_start(out=g[q4, 128:192, cs], in_=omT[sb][:]), reads=[("omT", sb)], writes=[("gin", q4, s4, 1)])
            fw.dma("pool", lambda e: e.dma_start(out=g[q4, 192:256, cs], in_=ocT[sb][:]), reads=[("ocT", sb)], writes=[("gin", q4, s4, 2)])
            if s4 == 3 and debug != 2:
                rk = [("gin", q4, i, j) for i in range(4) for j in range(3)]
                fw.coll(lambda e: e.collective_compute("AllGather", ALU.bypass, replica_groups=[[0, 1, 2, 3], [4, 5, 6, 7]],
                                                       ins=[g[q4]], outs=[gall.ap()[q4]]), reads=rk, writes=[("gall", q4)])
            yield

        def _chain(gens):
            for g_ in gens:
                yield from g_

        def _interleave(ga, gb):
            a_done, b_done = False, gb is None
            while not (a_done and b_done):
                if not a_done:
                    try:
                        next(ga)
                    except StopIteration:
                        a_done = True
                if not b_done:
                    try:
                        next(gb)
                    except StopIteration:
                        b_done = True

        prev = None
        ntile = 4 * nsb
        if ntile:
            for _ in tile_head(0):
                pass
        for s in range(nsb):
            _interleave(_chain([tile_step(4 * s + a, tile_head(4 * s + a + 1) if 4 * s + a + 1 < ntile else None) for a in range(4)]), prev)
            prev = attn_block(s)
        _interleave(iter(()), prev)
        if debug == 2:
            for s_ in range(nsb):
                q4, s4 = s_ // 4, s_ % 4
                rk = [("gin", q4, s4, j) for j in range(3)]
                fw.dma("pool", lambda e, q4=q4, s4=s4: e.dma_start(out=dbg2[q4, :, s4 * 512:(s4 + 1) * 512], in_=gin.ap()[q4, :, s4 * 512:(s4 + 1) * 512]),
                       reads=rk, writes=[("dbg2", s_)])
        if debug == 1:
            for q4 in range(4):
                fw.dma("pool", lambda e, q4=q4: e.dma_start(out=dbg[q4], in_=gall.ap()[q4]), reads=[("gall", q4)], writes=[("dbg", q4)])
        if fw.limit is not None:
            print("LAST OPS", fw.log[-3:])
        print("stage1 ops", fw.emit())

    if debug in (1, 2):
        return nc
    CAP = 640
    TPG = CAP // 128
    NST = 4 * TPG
    x1d = nc.dram_tensor("x1d", [TOK2, DM], F32)
    iota_d = din("iota_row", [1, CAP])
    sid_d = din("sid", [128, NST])
    goff_d = din("goff", [1, 4])
    es_o = contextlib.ExitStack()
    with es_o:
        def SO(name, shape, dt):
            return es_o.enter_context(nc.sbuf_tensor("s2_" + name, list(shape), dt))
        Wg = SO("Wg", [128, 16, 32], F32)
        slotf = SO("slotf", [128, 16], F32)
        slotrow = SO("slotrow", [128, TOK2], F32)
        Whl = SO("Whl", [128, 16, 4, 16], BF16)
        Wsort = SO("Wsort", [128, NST, 8], F32)
        h2s = SO("h2s", [128, 8, 4 * CAP], BF16)
        R2 = SO("R2", [128, NST, DM], BF16)
        xnb = R2
        ysall = R2

        fb = FW(nc, "b")
        es2 = contextlib.ExitStack()
        with es2:
            def S2(name, shape, dt):
                return es2.enter_context(nc.sbuf_tensor("s2a_" + name, list(shape), dt))

            def P2(name, shape, dt):
                return es2.enter_context(nc.psum_tensor("p2a_" + name, list(shape), dt))
            mixq = S2("mixq", [128, 8, TOK2], BF16)
            wo = S2("wo", [128, 8, DM], BF16)
            wr32 = S2("wr32", [128, 8, 36], F32)
            brb = S2("brb", [128, 36], F32)
            gff = S2("gff", [128, 8], F32)
            idf2 = S2("idf2", [128, 128], F32)
            tri2 = S2("tri2", [128, 128], F32)
            striu = S2("striu", [128, 128], BF16)
            ones2 = S2("ones2", [128, 128], F32)
            ones2b = S2("ones2b", [128, 128], BF16)
            goff = S2("goff", [128, 4], F32)
            gm_all = S2("gm_all", [128, 16, 4], BF16)
            diag = S2("diag", [128, 128], F32)
            tmp4 = S2("tmp4", [128, 4], F32)
            whi = S2("whi", [128, 32], BF16)
            wres = S2("wres", [128, 32], F32)
            xt = [S2("xt%d" % i, [128, DM], F32) for i in range(2)]
            x1t = [S2("x1t%d" % i, [128, DM], F32) for i in range(2)]
            xn2 = S2("xn2", [128, DM], F32)
            junk2 = S2("junk2", [128, DM], BF16)
            h32 = S2("h32", [128, 8, 128], F32)
            ss2 = S2("ss2", [128, 4], F32)
            lgb = S2("lgb", [128, 36], F32)
            gsm = S2("gsm", [128, 8], F32)
            eg = S2("eg", [128, 4], F32)
            gmask = S2("gmask", [128, 4], F32)
            m1 = S2("m1", [128, 4], F32)
            m2 = S2("m2", [128, 4], F32)
            eq1 = S2("eq1", [128, 4, 8], F32)
            eq2 = S2("eq2", [128, 4, 8], F32)
            le2 = S2("le2", [128, 4, 8], F32)
            dd = S2("dd", [128, 4], F32)
            ed = S2("ed", [128, 4], F32)
            w1 = S2("w1", [128, 4], F32)
            w2 = S2("w2", [128, 4], F32)
            gw = S2("gw", [128, 4], F32)
            tA = S2("tA", [128, 4, 8], F32)
            tB = S2("tB", [128, 4, 8], F32)
            PX = [P2("B0", [128, 512], F32), P2("B1", [128, 512], F32)]
            PA = [P2("B2", [128, 512], F32), P2("B3", [128, 512], F32)]
            PL = P2("B4", [128, 512], F32)
            PP = P2("B5", [128, 512], F32)
            PR = P2("B6", [128, 512], F32)

            pid = nc.gpsimd.partition_id()
            if debug == 3:
                gall_in = nc.dram_tensor("gall_in", [4, 1024, 2048], BF16, kind="ExternalInput").ap()
                fb.dma("pool", lambda e: e.dma_start(out=gall.ap(), in_=gall_in), writes=["gall"])
            for r in range(4):
                for hf in range(2):
                    fb.dma("pool", lambda e, r=r, hf=hf: e.dma_start(
                        out=mixq[:, 2 * r + hf, :],
                        in_=gall.ap()[bass.ds(pid % 4, 1), r * 256 + hf * 128:r * 256 + hf * 128 + 128, :].rearrange("a p n -> (a p) n")),
                        reads=["gall"], writes=[("mixq", 2 * r + hf)])
            fb.dma("pool", lambda e: e.dma_start(out=wo[:], in_=w_out_p.rearrange("(c p) n -> p c n", p=128)), writes=["wo"])
            fb.dma("sp", lambda e: e.dma_start(out=wr32[:], in_=w_r.rearrange("(c p) n -> p c n", p=128)), writes=["wr32"])
            fb.dma("sp", lambda e: e.dma_start(out=brb[:], in_=b_r.partition_broadcast(128)), writes=["brb"])
            fb.dma("sp", lambda e: e.dma_start(out=gff[:], in_=g_ffn), writes=["gff"])
            fb.dma("sp", lambda e: e.dma_start(out=idf2[:], in_=ident_d), writes=["idf2"])
            fb.dma("sp", lambda e: e.dma_start(out=tri2[:], in_=triu_d), writes=["tri2"])
            fb.dma("sp", lambda e: e.dma_start(out=goff[:], in_=goff_d.partition_broadcast(128)), writes=["goff"])
            fb.op("dve", lambda e: e.tensor_tensor(out=striu[:], in0=tri2[:], in1=idf2[:], op=ALU.subtract), reads=["tri2", "idf2"], writes=["striu"])
            fb.op("dve", lambda e: e.memset(ones2[:], 1.0), writes=["ones2"])
            fb.op("dve", lambda e: e.memset(ones2b[:], 1.0), writes=["ones2b"])
            mixkeys = [("mixq", i) for i in range(8)]

            def s2_head(tt):
                xi = tt % 2
                tk = ("x1t", xi)
                fb.dma("sp", lambda e: e.dma_start(out=xt[xi][:], in_=xo[tt * 128:(tt + 1) * 128, :]), writes=[("xt", xi)])
                for hf in range(2):
                    for c in range(8):
                        fb.op("pe", lambda e, c=c, hf=hf: e.matmul(PX[hf][:, :], lhsT=mixq[:, c, tt * 128:(tt + 1) * 128], rhs=wo[:, c, hf * 512:(hf + 1) * 512],
                                                                 start=(c == 0), stop=(c == 7)), reads=mixkeys + ["wo"], writes=["B%d" % hf])
                    fb.op("dve", lambda e, hf=hf: e.tensor_tensor(out=x1t[xi][:, hf * 512:(hf + 1) * 512], in0=PX[hf][:, :], in1=xt[xi][:, hf * 512:(hf + 1) * 512], op=ALU.add),
                          reads=["B%d" % hf, ("xt", xi)], writes=[tk])
                yield
                fb.dma("sp", lambda e: e.dma_start(out=x1d.ap()[tt * 128:(tt + 1) * 128, :], in_=x1t[xi][:]), reads=[tk], writes=[("x1d", tt)])
                fb.op("act", lambda e: e.activation(out=junk2[:], in_=x1t[xi][:], func=AF.Square, accum_out=ss2[:, 0:1]), reads=[tk], writes=["junk2", "ss2"])
                fb.op("act", lambda e: e.activation(out=ss2[:, 1:2], in_=ss2[:, 0:1], func=AF.Ln, scale=1.0 / DM, bias=EPS), reads=["ss2"], writes=["ss2b"])
                fb.op("act", lambda e: e.activation(out=ss2[:, 2:3], in_=ss2[:, 1:2], func=AF.Exp, scale=-0.5), reads=["ss2b"], writes=["ss2c"])
                fb.op("dve", lambda e: e.tensor_scalar(out=xn2[:], in0=x1t[xi][:], scalar1=ss2[:, 2:3], scalar2=None, op0=ALU.mult), reads=[tk, "ss2c"], writes=["xn2"])
                yield
                fb.op("act", lambda e: e.copy(out=xnb[:, tt, :], in_=xn2[:]), reads=["xn2"], writes=[("xnb", tt)])
                for c in range(8):
                    fb.op("pe", lambda e, c=c: e.transpose(out=PA[c // 4][:, (c % 4) * 128:(c % 4 + 1) * 128], in_=xn2[:, c * 128:(c + 1) * 128], identity=idf2[:]),
                          reads=["xn2", "idf2"], writes=["B%d" % (2 + c // 4)])
                yield
                for hf in range(2):
                    fb.op("dve", lambda e, hf=hf: e.tensor_tensor(out=h32[:, hf * 4:(hf + 1) * 4, :], in0=PA[hf][:, :].rearrange("p (c t) -> p c t", c=4),
                                                                in1=gff[:, hf * 4:(hf + 1) * 4].unsqueeze(2).to_broadcast([128, 4, 128]), op=ALU.mult),
                          reads=["B%d" % (2 + hf), "gff"], writes=[("h32", hf)])
                for c in range(8):
                    fb.op("pe", lambda e, c=c: e.matmul(PL[:, 0:36], lhsT=h32[:, c, :], rhs=wr32[:, c, :], start=(c == 0), stop=(c == 7)),
                          reads=[("h32", 0), ("h32", 1), "wr32"], writes=["B4"])
                yield

            def s2_tail(tt):
                D = lambda fn, r, w: fb.op("dve", fn, reads=r, writes=w)
                D(lambda e: e.tensor_tensor(out=lgb[:], in0=PL[:, 0:36], in1=brb[:], op=ALU.add), ["B4", "brb"], ["lgb"])
                D(lambda e: e.reduce_max(out=gsm[:, 0:1], in_=lgb[:, 0:4], axis=AX.X), ["lgb"], ["g0"])
                D(lambda e: e.tensor_scalar(out=gsm[:, 1:2], in0=gsm[:, 0:1], scalar1=-1.0, scalar2=None, op0=ALU.mult), ["g0"], ["g1"])
                fb.op("act", lambda e: e.activation(out=eg[:], in_=lgb[:, 0:4], func=AF.Exp, bias=gsm[:, 1:2], scale=1.0, accum_out=gsm[:, 2:3]),
                      reads=["lgb", "g1"], writes=["eg", "g2"])
                D(lambda e: e.reciprocal(out=gsm[:, 3:4], in_=gsm[:, 2:3]), ["g2"], ["g3"])
                D(lambda e: e.tensor_scalar(out=gmask[:], in0=lgb[:, 0:4], scalar1=gsm[:, 0:1], scalar2=None, op0=ALU.is_ge), ["lgb", "g0"], ["gmask"])
                le = lgb[:, 4:36].rearrange("p (g k) -> p g k", g=4)
                bc = lambda t: t[:, :].unsqueeze(2).to_broadcast([128, 4, 8])
                yield
                D(lambda e: e.reduce_max(out=m1[:], in_=le, axis=AX.X), ["lgb"], ["m1"])
                D(lambda e: e.tensor_tensor(out=eq1[:], in0=le, in1=bc(m1), op=ALU.is_ge), ["lgb", "m1"], ["eq1"])
                D(lambda e: e.scalar_tensor_tensor(out=le2[:], in0=eq1[:], scalar=-1e30, in1=le, op0=ALU.mult, op1=ALU.add), ["eq1", "lgb"], ["le2"])
                D(lambda e: e.reduce_max(out=m2[:], in_=le2[:], axis=AX.X), ["le2"], ["m2"])
                D(lambda e: e.tensor_tensor(out=eq2[:], in0=le2[:], in1=bc(m2), op=ALU.is_ge), ["le2", "m2"], ["eq2"])
                yield
                D(lambda e: e.tensor_tensor(out=dd[:], in0=m2[:], in1=m1[:], op=ALU.subtract), ["m1", "m2"], ["dd"])
                fb.op("act", lambda e: e.activation(out=ed[:], in_=dd[:], func=AF.Exp), reads=["dd"], writes=["ed"])
                D(lambda e: e.tensor_scalar(out=w1[:], in0=ed[:], scalar1=1.0, scalar2=None, op0=ALU.add), ["ed"], ["w1"])
                D(lambda e: e.reciprocal(out=w1[:], in_=w1[:]), ["w1"], ["w1"])
                D(lambda e: e.tensor_tensor(out=w2[:], in0=ed[:], in1=w1[:], op=ALU.mult), ["ed", "w1"], ["w2"])
                D(lambda e: e.tensor_scalar(out=gw[:], in0=gmask[:], scalar1=gsm[:, 3:4], scalar2=None, op0=ALU.mult), ["gmask", "g3"], ["gw"])
                D(lambda e: e.tensor_tensor(out=w1[:], in0=w1[:], in1=gw[:], op=ALU.mult), ["w1", "gw"], ["w1"])
                D(lambda e: e.tensor_tensor(out=w2[:], in0=w2[:], in1=gw[:], op=ALU.mult), ["w2", "gw"], ["w2"])
                yield
                D(lambda e: e.tensor_tensor(out=tA[:], in0=eq1[:], in1=bc(w1), op=ALU.mult), ["eq1", "w1"], ["tA"])
                D(lambda e: e.tensor_tensor(out=tB[:], in0=eq2[:], in1=bc(w2), op=ALU.mult), ["eq2", "w2"], ["tB"])
                D(lambda e: e.tensor_tensor(out=Wg[:, tt, :].rearrange("p (g k) -> p g k", g=4), in0=tA[:], in1=tB[:], op=ALU.add), ["tA", "tB"], [("Wg", tt)])
                yield
                fb.op("act", lambda e: e.copy(out=whi[:], in_=Wg[:, tt, :]), reads=[("Wg", tt)], writes=["whi"])
                D(lambda e: e.tensor_tensor(out=wres[:], in0=Wg[:, tt, :], in1=whi[:], op=ALU.subtract), [("Wg", tt), "whi"], ["wres"])
                fb.op("act", lambda e: e.copy(out=Whl[:, tt, :, 0:8], in_=whi[:, :].rearrange("p (g k) -> p g k", g=4)), reads=["whi"], writes=[("Whl", tt, 0)])
                fb.op("act", lambda e: e.copy(out=Whl[:, tt, :, 8:16], in_=wres[:, :].rearrange("p (g k) -> p g k", g=4)), reads=["wres"], writes=[("Whl", tt, 1)])
                yield
                fb.op("act", lambda e: e.copy(out=gm_all[:, tt, :], in_=gmask[:]), reads=["gmask"], writes=[("gm", tt)])
                for tp in range(tt):
                    fb.op("pe", lambda e, tp=tp: e.matmul(PP[:, 0:4], lhsT=ones2b[:], rhs=gm_all[:, tp, :], start=(tp == 0), stop=False),
                          reads=["ones2b", ("gm", tp)], writes=["B5"])
                fb.op("pe", lambda e: e.matmul(PP[:, 0:4], lhsT=striu[:], rhs=gm_all[:, tt, :], start=(tt == 0), stop=True), reads=["striu", ("gm", tt)], writes=["B5"])
                yield
                D(lambda e: e.tensor_tensor(out=tmp4[:], in0=PP[:, 0:4], in1=goff[:], op=ALU.add), ["B5", "goff"], ["tmp4"])
                D(lambda e: e.tensor_tensor(out=tmp4[:], in0=tmp4[:], in1=gmask[:], op=ALU.mult), ["tmp4", "gmask"], ["tmp4"])
                D(lambda e: e.reduce_sum(out=slotf[:, tt:tt + 1], in_=tmp4[:], axis=AX.X), ["tmp4"], [("slotf", tt)])
                yield
                D(lambda e: e.tensor_scalar(out=diag[:], in0=idf2[:], scalar1=slotf[:, tt:tt + 1], scalar2=None, op0=ALU.mult), ["idf2", ("slotf", tt)], ["diag"])
                fb.op("pe", lambda e: e.matmul(PR[:, 0:128], lhsT=ones2[:], rhs=diag[:], start=True, stop=True), reads=["ones2", "diag"], writes=["B6"])
                fb.op("act", lambda e: e.copy(out=slotrow[:, tt * 128:(tt + 1) * 128], in_=PR[:, 0:128]), reads=["B6"], writes=[("slotrow", tt)])
                yield

            for _ in s2_head(0):
                pass
            for tt in range(16):
                gens_ = [s2_tail(tt)] + ([s2_head(tt + 1)] if tt + 1 < 16 else [])
                while gens_:
                    for g_ in list(gens_):
                        try:
                            next(g_)
                        except StopIteration:
                            gens_.remove(g_)
            print("stage2a ops", fb.emit())

        CH = ((0, 384), (384, 256))
        fc1 = FW(nc, "c")
        es3 = contextlib.ExitStack()
        with es3:
            def S3(name, shape, dt):
                return es3.enter_context(nc.sbuf_tensor("s2c_" + name, list(shape), dt))

            def P3(name, shape, dt):
                return es3.enter_context(nc.psum_tensor("p2c_" + name, list(shape), dt))
            Ptb = S3("Ptb", [128, 16, 512], BF16)
            iota = S3("iota", [128, CAP], F32)
            slotrel = S3("slotrel", [128, 4, 16], F32)
            gff2 = S3("gff2", [128, 8], F32)
            wtmp = S3("wtmp", [128, 64], F32)
            PB = [P3("B%d" % i, [128, 512], F32) for i in range(4)]
            PW = P3("B4", [128, 512], F32)
            fc1.dma("sp", lambda e: e.dma_start(out=iota[:], in_=iota_d.partition_broadcast(128)), writes=["iota"])
            fc1.dma("sp", lambda e: e.dma_start(out=gff2[:], in_=g_ffn), writes=["gff2"])
            for g in range(4):
                fc1.op("dve", lambda e, g=g: e.tensor_scalar(out=slotrel[:, g, :], in0=slotf[:], scalar1=-1024.0 * g, scalar2=None, op0=ALU.add), writes=[("slotrel", g)])
            for g in range(4):
                for (off, n) in CH:
                    for tt in range(16):
                        fc1.op("dve", lambda e, g=g, off=off, n=n, tt=tt: e.tensor_scalar(out=Ptb[:, tt, 0:n], in0=iota[:, off:off + n], scalar1=slotrel[:, g, tt:tt + 1],
                                                                                     scalar2=None, op0=ALU.is_equal), reads=["iota", ("slotrel", g)], writes=[("Pt", tt)])
                    for c in range(8):
                        c4 = c % 4
                        for tt in range(16):
                            fc1.op("pe", lambda e, c=c, c4=c4, tt=tt, n=n: e.matmul(PB[c4][:, 0:n], lhsT=xnb[:, tt, c * 128:(c + 1) * 128], rhs=Ptb[:, tt, 0:n],
                                                                                 start=(tt == 0), stop=(tt == 15)), reads=[("Pt", tt)], writes=["B%d" % c4])
                        fc1.op("dve", lambda e, c=c, c4=c4, g=g, off=off, n=n: e.tensor_scalar(out=h2s[:, c, g * CAP + off:g * CAP + off + n], in0=PB[c4][:, 0:n],
                                                                                           scalar1=gff2[:, c:c + 1], scalar2=None, op0=ALU.mult),
                               reads=["B%d" % c4, "gff2"], writes=[("h2s", g, off, c)])
                    nj = n // 128
                    for j in range(nj):
                        for tt in range(16):
                            fc1.op("pe", lambda e, j=j, tt=tt, g=g: e.matmul(PW[:, j * 16:(j + 1) * 16], lhsT=Ptb[:, tt, j * 128:(j + 1) * 128], rhs=Whl[:, tt, g, :],
                                                                           start=(tt == 0), stop=(tt == 15)), reads=[("Pt", tt)], writes=["B4"])
                    st0 = g * TPG + off // 128
                    fc1.op("act", lambda e, nj=nj: e.copy(out=wtmp[:, 0:nj * 16], in_=PW[:, 0:nj * 16]), reads=["B4"], writes=["wtmp"])
                    fc1.op("dve", lambda e, nj=nj, st0=st0: e.tensor_tensor(out=Wsort[:, st0:st0 + nj, :], in0=wtmp[:, 0:nj * 16].rearrange("p (j k) -> p j k", j=nj)[:, :, 0:8],
                                                                         in1=wtmp[:, 0:nj * 16].rearrange("p (j k) -> p j k", j=nj)[:, :, 8:16], op=ALU.add),
                           reads=["wtmp"], writes=[("Wsort", st0)])
            print("stage2c1 ops", fc1.emit())

        fc = FW(nc, "d")
        es4 = contextlib.ExitStack()
        with es4:
            def S4(name, shape, dt):
                return es4.enter_context(nc.sbuf_tensor("s2d_" + name, list(shape), dt))

            def P4(name, shape, dt):
                return es4.enter_context(nc.psum_tensor("p2d_" + name, list(shape), dt))
            wgs = [S4("wg%d" % i, [128, 8, FF], BF16) for i in range(2)]
            wus = [S4("wu%d" % i, [128, 8, FF], BF16) for i in range(2)]
            wds = [S4("wd%d" % i, [128, 4, DM], BF16) for i in range(2)]
            sg = [S4("sg%d" % i, [128, 512], BF16) for i in range(2)]
            hid = [S4("hid%d" % i, [128, 4, 512], BF16) for i in range(2)]
            ys = S4("ys", [128, TPG, DM], F32)
            PG = [P4("B0", [128, 512], F32), P4("B1", [128, 512], F32)]
            PU = [P4("B2", [128, 512], F32), P4("B3", [128, 512], F32)]
            PY = [P4("B4", [128, 512], F32), P4("B5", [128, 512], F32)]
            cnt = 0
            for g in range(4):
                for e8 in range(8):
                    ex = g * 8 + e8
                    wb = ex % 2
                    fc.dma("pool", lambda e, ex=ex, wb=wb: e.dma_start(out=wgs[wb][:], in_=w_gate[ex]), writes=[("wg", wb)])
                    fc.dma("pool", lambda e, ex=ex, wb=wb: e.dma_start(out=wus[wb][:], in_=w_up[ex]), writes=[("wu", wb)])
                    fc.dma("pool", lambda e, ex=ex, wb=wb: e.dma_start(out=wds[wb][:], in_=w_down[ex]), writes=[("wd", wb)])
                    for (off, n) in CH:
                        hb = cnt % 2
                        cnt += 1
                        for f in range(4):
                            gb = f % 2
                            for c in range(8):
                                fc.op("pe", lambda e, c=c, f=f, gb=gb, wb=wb, g=g, off=off, n=n: e.matmul(
                                    PG[gb][:, 0:n], lhsT=wgs[wb][:, c, f * 128:(f + 1) * 128], rhs=h2s[:, c, g * CAP + off:g * CAP + off + n],
                                    start=(c == 0), stop=(c == 7)), reads=[("wg", wb)], writes=["B%d" % gb])
                            for c in range(8):
                                fc.op("pe", lambda e, c=c, f=f, gb=gb, wb=wb, g=g, off=off, n=n: e.matmul(
                                    PU[gb][:, 0:n], lhsT=wus[wb][:, c, f * 128:(f + 1) * 128], rhs=h2s[:, c, g * CAP + off:g * CAP + off + n],
                                    start=(c == 0), stop=(c == 7)), reads=[("wu", wb)], writes=["B%d" % (2 + gb)])
                            fc.op("act", lambda e, gb=gb, n=n: e.activation(out=sg[gb][:, 0:n], in_=PG[gb][:, 0:n], func=AF.Silu), reads=["B%d" % gb], writes=[("sg", gb)])
                            fc.op("dve", lambda e, gb=gb, hb=hb, f=f, n=n: e.tensor_tensor(out=hid[hb][:, f, 0:n], in0=PU[gb][:, 0:n], in1=sg[gb][:, 0:n], op=ALU.mult),
                                  reads=["B%d" % (2 + gb), ("sg", gb)], writes=[("hid", hb, f)])
                        for j in range(n // 128):
                            sl = off // 128 + j
                            for hf in range(2):
                                yb = hf
                                for f in range(4):
                                    fc.op("pe", lambda e, f=f, hb=hb, j=j, hf=hf, wb=wb, yb=yb: e.matmul(
                                        PY[yb][:, :], lhsT=hid[hb][:, f, j * 128:(j + 1) * 128], rhs=wds[wb][:, f, hf * 512:(hf + 1) * 512],
                                        start=(f == 0), stop=(f == 3)),
                                        reads=[("hid", hb, 0), ("hid", hb, 1), ("hid", hb, 2), ("hid", hb, 3), ("wd", wb)], writes=["B%d" % (4 + yb)])
                                if e8 == 0:
                                    fc.op("dve", lambda e, yb=yb, sl=sl, hf=hf, g=g, e8=e8: e.tensor_scalar(
                                        out=ys[:, sl, hf * 512:(hf + 1) * 512], in0=PY[yb][:, :], scalar1=Wsort[:, g * TPG + sl, e8:e8 + 1], scalar2=None, op0=ALU.mult),
                                        reads=["B%d" % (4 + yb)], writes=[("ys", sl, hf)])
                                else:
                                    fc.op("dve", lambda e, yb=yb, sl=sl, hf=hf, g=g, e8=e8: e.scalar_tensor_tensor(
                                        out=ys[:, sl, hf * 512:(hf + 1) * 512], in0=PY[yb][:, :], scalar=Wsort[:, g * TPG + sl, e8:e8 + 1],
                                        in1=ys[:, sl, hf * 512:(hf + 1) * 512], op0=ALU.mult, op1=ALU.add),
                                        reads=["B%d" % (4 + yb)], writes=[("ys", sl, hf)])
                for sl in range(TPG):
                    fc.op("act", lambda e, sl=sl, g=g: e.copy(out=ysall[:, g * TPG + sl, :], in_=ys[:, sl, :]), reads=[("ys", sl, 0), ("ys", sl, 1)], writes=[("ysall", g, sl)])
            print("stage2c2 ops", fc.emit())

        fu = FW(nc, "e")
        es5 = contextlib.ExitStack()
        with es5:
            def S5(name, shape, dt):
                return es5.enter_context(nc.sbuf_tensor("s2e_" + name, list(shape), dt))

            def P5(name, shape, dt):
                return es5.enter_context(nc.psum_tensor("p2e_" + name, list(shape), dt))
            sid = S5("sid", [128, NST], F32)
            PT = [S5("PT%d" % i, [128, NST, 128], BF16) for i in range(2)]
            x1r = [S5("x1r%d" % i, [128, DM], F32) for i in range(2)]
            ot = [S5("ot%d" % i, [128, DM], F32) for i in range(2)]
            PUo = [P5("B%d" % i, [128, 512], F32) for i in range(4)]
            fu.dma("sp", lambda e: e.dma_start(out=sid[:], in_=sid_d), writes=["sid"])
            for tt in range(16):
                xi = tt % 2
                fu.dma("pool", lambda e, tt=tt, xi=xi: e.dma_start(out=x1r[xi][:], in_=x1d.ap()[tt * 128:(tt + 1) * 128, :]), writes=[("x1r", xi)])
                for st in range(NST):
                    eng = "dve" if st % 2 == 0 else "pool"
                    fu.op(eng, lambda e, st=st, tt=tt, xi=xi: e.tensor_scalar(out=PT[xi][:, st, :], in0=slotrow[:, tt * 128:(tt + 1) * 128], scalar1=sid[:, st:st + 1],
                                                                          scalar2=None, op0=ALU.is_equal), reads=["sid"], writes=[("PT", xi, st)])
                for hf in range(2):
                    pb = (tt * 2 + hf) % 4
                    for st in range(NST):
                        fu.op("pe", lambda e, st=st, hf=hf, pb=pb, xi=xi: e.matmul(PUo[pb][:, :], lhsT=PT[xi][:, st, :], rhs=ysall[:, st, hf * 512:(hf + 1) * 512],
                                                                              start=(st == 0), stop=(st == NST - 1)), reads=[("PT", xi, st)], writes=["B%d" % pb])
                    fu.op("dve", lambda e, hf=hf, pb=pb, xi=xi: e.tensor_tensor(out=ot[xi][:, hf * 512:(hf + 1) * 512], in0=PUo[pb][:, :], in1=x1r[xi][:, hf * 512:(hf + 1) * 512], op=ALU.add),
                          reads=["B%d" % pb, ("x1r", xi)], writes=[("ot", xi, hf)])
                fu.dma("sp", lambda e, tt=tt, xi=xi: e.dma_start(out=out[tt * 128:(tt + 1) * 128, :], in_=ot[xi][:]), reads=[("ot", xi, 0), ("ot", xi, 1)], writes=[("out", tt)])
            print("stage2c3 ops", fu.emit())
    return nc


def make_inputs(inputs):
    f = lambda a: np.ascontiguousarray(np.asarray(a, dtype=np.float32))
    x = f(inputs["x"]); mem = f(inputs["mem"])
    w_in = f(inputs["w_in"])[0]
    o_gq, o_gk, o_gv, o_gr, o_lr, o_mq, o_mk, o_mv, o_cq = 0, 256, 512, 1024, 1536, 1552, 1808, 2064, 2320
    gcol = lambda g: f(inputs[g])[0].reshape(8, 128).T.copy()
    w_out = f(inputs["w_out"])[0]
    perm = []
    for h in range(4):
        perm += list(range(128 * h, 128 * h + 128)) + list(range(512 + 64 * h, 512 + 64 * h + 64)) + list(range(768 + 64 * h, 768 + 64 * h + 64))
    w_out_p = np.ascontiguousarray(w_out[perm])
    w_r = np.ascontiguousarray(np.concatenate([f(inputs["w_router_group"])[0]] + [f(inputs["w_router_expert"])[0][g] for g in range(4)], axis=1))
    b_r = np.concatenate([f(inputs["b_router_group"])[0], f(inputs["b_router_expert"])[0].reshape(-1)])[None, :].copy()
    w_gate = np.ascontiguousarray(f(inputs["w_gate"])[0].reshape(NEXP, 8, 128, FF).transpose(0, 2, 1, 3))
    w_up = np.ascontiguousarray(f(inputs["w_up"])[0].reshape(NEXP, 8, 128, FF).transpose(0, 2, 1, 3))
    w_down = np.ascontiguousarray(f(inputs["w_down"])[0].reshape(NEXP, 4, 128, DM).transpose(0, 2, 1, 3))
    ident = np.eye(128, dtype=np.float32)
    triu = np.triu(np.ones((128, 128), np.float32))
    t = np.arange(SEQ)
    maps = []
    for c in range(8):
        b, h = c // 4, c % 4
        m = 2.0 ** (-2.0 * (h + 1))
        cols_a = np.concatenate([np.arange(o_gq + 64 * h, o_gq + 64 * h + 64), np.arange(o_gk + 64 * h, o_gk + 64 * h + 64),
                                 np.arange(o_gv + 128 * h, o_gv + 128 * h + 128), np.arange(o_gr + 128 * h, o_gr + 128 * h + 128),
                                 np.arange(o_mq + 64 * h, o_mq + 64 * h + 64), np.arange(o_mk + 64 * h, o_mk + 64 * h + 64),
                                 np.arange(o_mv + 64 * h, o_mv + 64 * h + 64), np.arange(o_cq + 64 * h, o_cq + 64 * h + 64)])
        kaug = np.zeros((34, SEQ), np.float32)
        kaug[t // 256, t] = 1.0
        kaug[32] = m * (t % 256)
        kaug[33] = 1.0
        qaug = np.zeros((2, 512), np.float32)
        qaug[0] = 1.0
        qaug[1] = -m * (np.arange(512) % 256)
        cb = np.full((32, 32), -BIG, np.float32)
        for own in range(32):
            for n in range(own):
                cb[own, n] = -BIG - m * 256.0 * (own - n)
            cb[own, own] = 0.0
        cmm = np.zeros((4, 128, 512), np.float32)
        for cc in range(4):
            for aa in range(4):
                if cc // 2 != aa // 2:
                    continue
                if aa == cc:
                    blk = np.where(np.arange(128)[:, None] <= np.arange(128)[None, :], 0.0, -BIG)
                elif aa < cc:
                    blk = np.full((128, 128), -BIG)
                else:
                    blk = np.zeros((128, 128))
                cmm[cc, :, aa * 128:(aa + 1) * 128] = blk
        cmm = np.ascontiguousarray(cmm.transpose(1, 0, 2).reshape(128, 2048))
        gq = np.concatenate([f(inputs["moba_q_norm_g"])[0], f(inputs["moba_k_norm_g"])[0], f(inputs["mem_q_norm_g"])[0], f(inputs["mem_k_norm_g"])[0]])[None, :].copy()
        maps.append({
            "xb": x[b], "xo": np.ascontiguousarray(x[b, TOK2 * h:TOK2 * (h + 1)]), "memb": mem[b],
            "w_in_ab": np.ascontiguousarray(w_in[:, cols_a]),
            "w_in_lrT": np.ascontiguousarray(w_in[:, o_lr:o_lr + 16].T),
            "w_gk": np.ascontiguousarray(f(inputs["w_gla_gk"])[0][:, 64 * h:64 * h + 64]),
            "b_gk": np.ascontiguousarray(f(inputs["b_gla_gk"])[0][64 * h:64 * h + 64][None, :]),
            "g_attn": gcol("attn_norm_g"), "g_mem": gcol("mem_norm_g"), "g_ffn": gcol("ffn_norm_g"),
            "g_gla": f(inputs["gla_out_norm_g"])[0][:, None].copy(),
            "g_qk": gq,
            "w_memkv": np.ascontiguousarray(np.concatenate([f(inputs["w_mem_kv"])[0][:, 64 * h:64 * h + 64],
                                                            f(inputs["w_mem_kv"])[0][:, 256 + 64 * h:256 + 64 * h + 64]], axis=1)),
            "w_out_p": w_out_p, "w_r": w_r, "b_r": b_r, "w_gate": w_gate, "w_up": w_up, "w_down": w_down,
            "iota_row": np.arange(640, dtype=np.float32)[None, :].copy(),
            "sid": np.ascontiguousarray((np.arange(128, dtype=np.float32)[:, None] + np.array([(st // 5) * 1024 + (st % 5) * 128 for st in range(20)], np.float32)[None, :])),
            "goff": np.array([[0.0, 1024.0, 2048.0, 3072.0]], np.float32),
            "kaug_c": kaug, "qaug_c": qaug, "cbt": cb.reshape(1, 1024).copy(), "cm": cmm, "ident": ident, "triu": triu,
        })
    return maps


_NC_CACHE = {}


def kernel(**inputs):
    maps = make_inputs(inputs)
    if "nc" not in _NC_CACHE:
        _NC_CACHE["nc"] = build_program(0)
    res = run_bass_kernel_spmd(_NC_CACHE["nc"], maps, core_ids=list(range(8)))
    outp = np.zeros((2, SEQ, DM), np.float32)
    for c in range(8):
        b, h = c // 4, c % 4
        outp[b, TOK2 * h:TOK2 * (h + 1)] = res.results[c]["out"]
    return outp
```

```python
import contextlib
import numpy as np
import concourse.bass as bass
import concourse.mybir as mybir
from concourse.bass_utils import run_bass_kernel_spmd

F32 = mybir.dt.float32
BF16 = mybir.dt.bfloat16
AF = mybir.ActivationFunctionType
ALU = mybir.AluOpType
AX = mybir.AxisListType

SEQ = 8192
DM = 1024
NT = SEQ // 128
NSB = SEQ // 512
EPS = 1e-6
BIG = 30000.0
NEXP = 32
FF = 512
TOK2 = 2048


class FW:
    ENG = ("pe", "act", "dve", "pool", "sp")
    EMAP = {"pe": "tensor", "act": "scalar", "dve": "vector", "pool": "gpsimd", "sp": "sync"}

    def __init__(self, nc, tag="", n_dma_sems=20):
        self.nc = nc
        self.ops = {e: [] for e in self.ENG}
        self.sem = {e: nc.alloc_semaphore("c%s_%s" % (tag, e)) for e in self.ENG}
        self.cnt = {e: 0 for e in self.ENG}
        nd = {"sp": 6, "pool": 4}
        self.dsems = {q: [nc.alloc_semaphore("d%s%s_%d" % (tag, q, i)) for i in range(n)] for q, n in nd.items()}
        self.dcnt = {q: [0] * n for q, n in nd.items()}
        self.dnext = {q: 0 for q in nd}
        self.known = {e: {} for e in self.ENG}
        self.lastw = {}
        self.readers = {}
        self.tag = tag
        self.ncoll = 0
        self.limit = None
        self.nops = 0
        self.log = []

    @staticmethod
    def _is_psum(k):
        if isinstance(k, tuple):
            k = k[0]
        return isinstance(k, str) and (k in ("PO", "PS") or (len(k) == 2 and k[0] == "B" and k[1].isdigit()))

    def _skip(self, eng, reads, writes):
        self.nops += 1
        if self.limit is not None and self.nops > self.limit:
            return True
        self.log.append((self.nops, eng, tuple(reads), tuple(writes)))
        return False

    def _deps(self, eng, reads, writes):
        deps = []
        for k in reads:
            if k in self.lastw:
                deps.append(self.lastw[k])
        for k in writes:
            if k in self.lastw:
                deps.append(self.lastw[k])
            deps.extend(self.readers.get(k, []))
        waits = {}
        for (sem, val, deng) in deps:
            if deng == eng and eng == "pe":
                continue
            kn = self.known[eng].get(sem.name, 0)
            if val <= kn:
                continue
            if sem.name not in waits or waits[sem.name][1] < val:
                waits[sem.name] = (sem, val)
        for name, (sem, val) in waits.items():
            self.known[eng][name] = val
        return list(waits.values())

    def _record(self, reads, writes, tok):
        for k in writes:
            self.lastw[k] = tok
            self.readers[k] = []
        for k in reads:
            self.readers.setdefault(k, []).append(tok)

    def op(self, eng, fn, reads=(), writes=()):
        if self._skip(eng, reads, writes):
            return
        ex = [k for k in reads if self._is_psum(k) and k not in writes]
        if ex:
            writes = list(writes) + ex
        waits = self._deps(eng, reads, writes)
        self.cnt[eng] += 1
        tok = (self.sem[eng], self.cnt[eng], eng)
        self.ops[eng].append((waits, fn, self.sem[eng], 1))
        self._record(reads, writes, tok)

    def dma(self, eng, fn, reads=(), writes=()):
        if self._skip(eng, reads, writes):
            return
        waits = self._deps(eng, reads, writes)
        i = self.dnext[eng]
        self.dnext[eng] = (i + 1) % len(self.dsems[eng])
        sem = self.dsems[eng][i]
        prev = self.dcnt[eng][i]
        if prev > 0 and self.known[eng].get(sem.name, 0) < prev:
            waits.append((sem, prev))
            self.known[eng][sem.name] = prev
        self.dcnt[eng][i] += 16
        tok = (sem, self.dcnt[eng][i], "dma")
        self.ops[eng].append((waits, fn, sem, 16))
        self._record(reads, writes, tok)

    def coll(self, fn, reads=(), writes=()):
        waits = self._deps("pool", reads, writes)
        sem = self.nc.alloc_semaphore("cc%s_%d" % (self.tag, self.ncoll))
        self.ncoll += 1
        tok = (sem, 1, "coll")
        self.ops["pool"].append((waits, fn, sem, 1))
        self._record(reads, writes, tok)

    def final_wait(self, eng):
        keys = list(self.lastw.keys())
        waits = self._deps(eng, keys, keys)
        self.ops[eng].append((waits, None, None, 0))

    def emit(self):
        nc = self.nc
        for e in ("sp", "pool", "act"):
            self.final_wait(e)
        with nc.Block() as block:
            for e in self.ENG:
                ops = self.ops[e]

                def body(engine, ops=ops):
                    for (waits, fn, sem, inc) in ops:
                        for (ws, wv) in waits:
                            engine.wait_ge(ws, wv)
                        if fn is not None:
                            ins = fn(engine)
                            ins.then_inc(sem, inc)
                getattr(block, self.EMAP[e])(body)
        n = {e: len(self.ops[e]) for e in self.ENG}
        self.ops = {e: [] for e in self.ENG}
        return n


def build_program(debug=0, nsb=NSB, n_exp=NEXP):
    nc = bass.Bass("TRN2", target_bir_lowering=False)

    def din(name, shape, dt=F32):
        return nc.dram_tensor(name, list(shape), dt, kind="ExternalInput").ap()

    xb = din("xb", [SEQ, DM])
    xo = din("xo", [TOK2, DM])
    memb = din("memb", [256, DM])
    w_in_ab = din("w_in_ab", [DM, 640])
    w_in_lrT = din("w_in_lrT", [16, DM])
    w_gk = din("w_gk", [16, 64])
    b_gk = din("b_gk", [1, 64])
    g_attn = din("g_attn", [128, 8])
    g_mem = din("g_mem", [128, 8])
    g_ffn = din("g_ffn", [128, 8])
    g_gla = din("g_gla", [128, 1])
    g_qk = din("g_qk", [1, 256])
    w_memkv = din("w_memkv", [DM, 128])
    w_out_p = din("w_out_p", [DM, DM])
    w_r = din("w_r", [DM, 36])
    b_r = din("b_r", [1, 36])
    if debug != 2:
        w_gate = din("w_gate", [NEXP, 128, 8, FF])
        w_up = din("w_up", [NEXP, 128, 8, FF])
        w_down = din("w_down", [NEXP, 128, 4, DM])
    kaug_c = din("kaug_c", [34, SEQ])
    qaug_c = din("qaug_c", [2, 512])
    cbt_d = din("cbt", [1, 1024])
    cm_d = din("cm", [128, 2048])
    ident_d = din("ident", [128, 128])
    triu_d = din("triu", [128, 128])
    out = nc.dram_tensor("out", [TOK2, DM], F32, kind="ExternalOutput").ap()
    dbg = None
    if debug:
        dbg = nc.dram_tensor("dbg_mix", [4, 1024, 2048], BF16, kind="ExternalOutput").ap()
        dbg2 = nc.dram_tensor("dbg_gin", [4, 256, 2048], BF16, kind="ExternalOutput").ap()

    gin = nc.dram_tensor("gin", [4, 256, 2048], BF16)
    gall = nc.dram_tensor("gall", [4, 1024, 2048], BF16)

    fw = FW(nc, "a")
    import os
    if os.environ.get("FW_LIMIT"):
        fw.limit = int(os.environ["FW_LIMIT"])
    es = contextlib.ExitStack()
    with es:
        def S(name, shape, dt):
            return es.enter_context(nc.sbuf_tensor("s1_" + name, list(shape), dt))

        def P(name, shape, dt):
            return es.enter_context(nc.psum_tensor("p1_" + name, list(shape), dt))

        idf = S("idf", [128, 128], F32)
        idb = S("idb", [128, 128], BF16)
        triu = S("triu", [128, 128], F32)
        ones32 = S("ones32", [128, 128], F32)
        onesb = S("onesb", [128, 128], BF16)
        w_in = S("w_in", [128, 8, 704], BF16)
        lrT = S("lrT", [16, DM], F32)
        wgk = S("wgk", [16, 64], F32)
        bgk = S("bgk", [128, 64], F32)
        gat = S("gat", [128, 8], F32)
        gme = S("gme", [128, 8], F32)
        ggla = S("ggla", [128, 1], F32)
        gqk = S("gqk", [128, 256], F32)
        wmkv = S("wmkv", [128, 8, 128], BF16)
        KT = S("KT", [128, SEQ], BF16)
        VA = S("VA", [128, NT, 128], BF16)
        QT = [S("QT%d" % i, [128, 512], BF16) for i in range(2)]
        CQT = [S("CQT%d" % i, [64, 512], BF16) for i in range(2)]
        cbt = S("cbt", [128, 1024], F32)
        cm = S("cm", [128, 2048], BF16)
        MKT = S("MKT", [64, 256], BF16)
        MVA = S("MVA", [128, 2, 128], BF16)
        gatebuf = S("gatebuf", [128, 32], F32)
        ksum = S("ksum", [64, NT], F32)
        kmT = S("kmT", [64, 32], F32)
        Sst = S("Sst", [64, 128], F32)
        Sbf = S("Sbf", [64, 128], BF16)
        xs = [S("xs%d" % i, [128, DM], F32) for i in range(2)]
        junk = S("junk", [128, DM], BF16)
        ss = S("ss", [128, 4], F32)
        rs = S("rs", [128, 4], F32)
        xn = S("xn", [128, DM], BF16)
        pj1s = [S("pj1s%d" % i, [128, 512], F32) for i in range(2)]
        pj2s = [S("pj2s%d" % i, [128, 192], F32) for i in range(2)]
        hT = S("hT", [128, 8, 128], BF16)
        ssq = S("ssq", [128, 4], F32)
        rs3 = S("rs3", [128, 4], F32)
        qn32 = S("qn32", [128, 64], F32)
        kn32 = S("kn32", [128, 64], F32)
        cqn = S("cqn", [128, 64], BF16)
        QT32 = S("QT32", [64, 128], F32)
        max8 = S("max8", [128, 8], F32)
        selpen = S("selpen", [128, 32], F32)
        qs = S("qs", [128, 96], BF16)
        zz = S("zz", [128, 64], F32)
        e1 = S("e1", [128, 64], F32)
        ll = S("ll", [128, 64], F32)
        ecum = S("ecum", [128, 64], F32)
        einv = S("einv", [128, 64], F32)
        eL = S("eL", [128, 64], F32)
        decay = S("decay", [64, 1], F32)
        kinv32 = S("kinv32", [128, 64], F32)
        qk2 = S("qk2", [128, 128], BF16)
        kend = S("kend", [128, 64], BF16)
        qkT = S("qkT", [64, 256], BF16)
        ATm = S("ATm", [128, 128], BF16)
        vg = S("vg", [128, 128], BF16)
        sq = S("sq", [128, 128], BF16)
        lnt = S("lnt", [128, 128], F32)
        rstdb = S("rstdb", [128, 128], F32)
        e2 = S("e2", [128, 128], F32)
        sr = S("sr", [128, 128], BF16)
        t1 = S("t1", [128, 128], F32)
        ogT = [S("ogT%d" % i, [128, 512], BF16) for i in range(2)]
        omT = [S("omT%d" % i, [64, 512], BF16) for i in range(2)]
        ocT = [S("ocT%d" % i, [64, 512], BF16) for i in range(2)]
        pexp = [S("pexp%d" % i, [128, 512], BF16) for i in range(3)]
        rden = S("rden", [64, 512], F32)
        memx = S("memx", [128, DM], F32)
        B0 = P("B0", [128, 1024], BF16)
        B1 = P("B1", [128, 512], F32)
        B2 = P("B2", [128, 512], F32)
        B3 = P("B3", [128, 512], F32)
        B4 = P("B4", [128, 512], F32)
        PS = [P("PS0", [128, 512], F32), P("PS1", [128, 512], F32)]
        PO = P("PO", [128, 512], F32)
        B4h = B2[:, 192:512].bitcast(BF16)

        fw.dma("sp", lambda e: e.dma_start(out=idf[:], in_=ident_d), writes=["idf"])
        fw.dma("pool", lambda e: e.dma_start(out=idb[:], in_=ident_d), writes=["idb"])
        fw.dma("sp", lambda e: e.dma_start(out=triu[:], in_=triu_d), writes=["triu"])
        fw.op("dve", lambda e: e.memset(ones32[:], 1.0), writes=["ones32"])
        fw.op("dve", lambda e: e.memset(onesb[:], 1.0), writes=["onesb"])
        fw.dma("pool", lambda e: e.dma_start(out=w_in[:, :, 0:640], in_=w_in_ab.rearrange("(c p) n -> p c n", p=128)), writes=["w_in"])
        fw.dma("sp", lambda e: e.dma_start(out=lrT[:], in_=w_in_lrT), writes=["lrT"])
        fw.dma("sp", lambda e: e.dma_start(out=wgk[:], in_=w_gk), writes=["wgk"])
        fw.dma("sp", lambda e: e.dma_start(out=bgk[:], in_=b_gk.partition_broadcast(128)), writes=["bgk"])
        fw.dma("sp", lambda e: e.dma_start(out=gat[:], in_=g_attn), writes=["gat"])
        fw.dma("sp", lambda e: e.dma_start(out=gme[:], in_=g_mem), writes=["gme"])
        fw.dma("sp", lambda e: e.dma_start(out=ggla[:], in_=g_gla), writes=["ggla"])
        fw.dma("sp", lambda e: e.dma_start(out=gqk[:], in_=g_qk.partition_broadcast(128)), writes=["gqk"])
        fw.dma("pool", lambda e: e.dma_start(out=wmkv[:], in_=w_memkv.rearrange("(c p) n -> p c n", p=128)), writes=["wmkv"])
        fw.dma("pool", lambda e: e.dma_start(out=KT[64:98, :], in_=kaug_c), writes=["KTc"])
        for i in range(2):
            fw.dma("pool", lambda e, i=i: e.dma_start(out=QT[i][96:98, :], in_=qaug_c), writes=[("QT", i)])
        fw.dma("sp", lambda e: e.dma_start(out=cbt[:], in_=cbt_d.partition_broadcast(128)), writes=["cbt"])
        fw.dma("pool", lambda e: e.dma_start(out=cm[:], in_=cm_d), writes=["cm"])
        fw.op("dve", lambda e: e.memset(VA[:, :, 64:128], 1.0), writes=["VAc"])
        fw.op("dve", lambda e: e.memset(MVA[:, :, 64:128], 1.0), writes=["MVA"])
        fw.op("dve", lambda e: e.memset(gatebuf[:], -1e30), writes=["gatebuf"])
        fw.op("dve", lambda e: e.memset(Sst[:], 0.0), writes=["Sst"])
        fw.op("dve", lambda e: e.memset(Sbf[:], 0.0), writes=["Sbf"])
        fw.op("dve", lambda e: e.memset(selpen[:], 0.0), writes=["selpen"])
        fw.op("dve", lambda e: e.tensor_scalar(out=gqk[:, 0:64], in0=gqk[:, 0:64], scalar1=0.125, scalar2=None, op0=ALU.mult), reads=["gqk"], writes=["gqk"])
        fw.op("dve", lambda e: e.tensor_scalar(out=gqk[:, 128:192], in0=gqk[:, 128:192], scalar1=0.125, scalar2=None, op0=ALU.mult), reads=["gqk"], writes=["gqk"])
        for c in range(8):
            fw.op("pe", lambda e, c=c: e.matmul(B1[:, c * 64:(c + 1) * 64], lhsT=lrT[:, c * 128:(c + 1) * 128], rhs=wgk[:], start=True, stop=True),
                  reads=["lrT", "wgk"], writes=["B1"])
        fw.op("dve", lambda e: e.tensor_copy(out=w_in[:, :, 640:704], in_=B1[:, 0:512].rearrange("p (c k) -> p c k", c=8)), reads=["B1"], writes=["w_in"])

        def rms_rows(src_key, src_ap, n, dst_rs):
            fw.op("act", lambda e: e.activation(out=junk[:, 0:n], in_=src_ap, func=AF.Square, accum_out=ss[:, 0:1]), reads=[src_key], writes=["junk", "ss"])
            fw.op("act", lambda e: e.activation(out=rs[:, 0:1], in_=ss[:, 0:1], func=AF.Ln, scale=1.0 / n, bias=EPS), reads=["ss"], writes=["rs"])
            fw.op("act", lambda e: e.activation(out=dst_rs, in_=rs[:, 0:1], func=AF.Exp, scale=-0.5), reads=["rs"], writes=["rs1"])

        def norm_and_transpose(x_key, x_ap, gains):
            rms_rows(x_key, x_ap, DM, rs[:, 1:2])
            fw.op("dve", lambda e: e.tensor_scalar(out=xn[:], in0=x_ap, scalar1=rs[:, 1:2], scalar2=None, op0=ALU.mult), reads=[x_key, "rs1"], writes=["xn"])
            for c in range(8):
                fw.op("pe", lambda e, c=c: e.transpose(out=B0[:, c * 128:(c + 1) * 128], in_=xn[:, c * 128:(c + 1) * 128], identity=idb[:]),
                      reads=["xn", "idb"], writes=["B0"])
            fw.op("dve", lambda e: e.tensor_tensor(out=hT[:], in0=B0[:, :].rearrange("p (c t) -> p c t", c=8),
                                                   in1=gains[:, :].unsqueeze(2).to_broadcast([128, 8, 128]), op=ALU.mult),
                  reads=["B0", "gat", "gme"], writes=["hT"])

        for mt in range(2):
            fw.dma("sp", lambda e, mt=mt: e.dma_start(out=memx[:], in_=memb[mt * 128:(mt + 1) * 128, :]), writes=["memx"])
            norm_and_transpose("memx", memx[:], gme)
            for c in range(8):
                fw.op("pe", lambda e, c=c: e.matmul(B2[:, 0:128], lhsT=hT[:, c, :], rhs=wmkv[:, c, :], start=(c == 0), stop=(c == 7)),
                      reads=["hT", "wmkv"], writes=["B2"])
            fw.op("act", lambda e: e.activation(out=junk[:, 0:64], in_=B2[:, 0:64], func=AF.Square, accum_out=ssq[:, 0:1]), reads=["B2"], writes=["junk", "ssq"])
            fw.op("act", lambda e: e.activation(out=rs3[:, 0:1], in_=ssq[:, 0:1], func=AF.Ln, scale=1.0 / 64, bias=EPS), reads=["ssq"], writes=["rs3"])
            fw.op("act", lambda e: e.activation(out=rs3[:, 0:1], in_=rs3[:, 0:1], func=AF.Exp, scale=-0.5), reads=["rs3"], writes=["rs3"])
            fw.op("dve", lambda e: e.scalar_tensor_tensor(out=cqn[:], in0=B2[:, 0:64], scalar=rs3[:, 0:1], in1=gqk[:, 192:256], op0=ALU.mult, op1=ALU.mult),
                  reads=["B2", "rs3", "gqk"], writes=["cqn"])
            fw.op("pe", lambda e: e.transpose(out=B4h[0:64, 0:128], in_=cqn[:], identity=idb[:]), reads=["cqn", "idb"], writes=["B2"])
            fw.op("dve", lambda e, mt=mt: e.tensor_copy(out=MKT[:, mt * 128:(mt + 1) * 128], in_=B4h[0:64, 0:128]), reads=["B2"], writes=["MKT"])
            fw.op("act", lambda e, mt=mt: e.copy(out=MVA[:, mt, 0:64], in_=B2[:, 64:128]), reads=["B2"], writes=["MVA"])

        def tile_head(tt):
            xi = tt % 2
            x_key, x_ap = ("xs", xi), xs[xi][:]
            fw.dma("sp", lambda e: e.dma_start(out=xs[xi][:], in_=xb[tt * 128:(tt + 1) * 128, :]), writes=[("xs", xi)])
            yield
            rms_rows(x_key, x_ap, DM, rs[:, 1:2])
            yield
            fw.op("dve", lambda e: e.tensor_scalar(out=xn[:], in0=x_ap, scalar1=rs[:, 1:2], scalar2=None, op0=ALU.mult), reads=[x_key, "rs1"], writes=["xn"])
            yield
            for c in range(8):
                fw.op("pe", lambda e, c=c: e.transpose(out=B0[:, c * 128:(c + 1) * 128], in_=xn[:, c * 128:(c + 1) * 128], identity=idb[:]),
                      reads=["xn", "idb"], writes=["B0"])
                if c % 4 == 3:
                    yield
            fw.op("dve", lambda e: e.tensor_tensor(out=hT[:], in0=B0[:, :].rearrange("p (c t) -> p c t", c=8),
                                                   in1=gat[:, :].unsqueeze(2).to_broadcast([128, 8, 128]), op=ALU.mult),
                  reads=["B0", "gat", "gme"], writes=["hT"])
            yield
            for c in range(8):
                fw.op("pe", lambda e, c=c: e.matmul(B1[:, 0:512], lhsT=hT[:, c, :], rhs=w_in[:, c, 0:512], start=(c == 0), stop=(c == 7)),
                      reads=["hT", "w_in"], writes=["B1"])
            for c in range(8):
                fw.op("pe", lambda e, c=c: e.matmul(B2[:, 0:192], lhsT=hT[:, c, :], rhs=w_in[:, c, 512:704], start=(c == 0), stop=(c == 7)),
                      reads=["hT", "w_in"], writes=["B2"])
            yield
            fw.op("act", lambda e: e.copy(out=pj1s[xi][:], in_=B1[:, 0:512]), reads=["B1"], writes=[("pj1", xi)])
            fw.op("dve", lambda e: e.tensor_copy(out=pj2s[xi][:], in_=B2[:, 0:192]), reads=["B2"], writes=[("pj2", xi)])
            yield

        def tile_step(tt, nxt=None):
            s, a = tt // 4, tt % 4
            sb = s % 2
            xi = tt % 2
            own = tt // 2
            fw.op("act", lambda e: e.activation(out=junk[:, 0:64], in_=pj1s[xi][:, 384:448], func=AF.Square, accum_out=ssq[:, 0:1]), reads=[("pj1", xi)], writes=["junk", "ssq"])
            fw.op("act", lambda e: e.activation(out=junk[:, 0:64], in_=pj1s[xi][:, 448:512], func=AF.Square, accum_out=ssq[:, 1:2]), reads=[("pj1", xi)], writes=["junk", "ssq"])
            fw.op("act", lambda e: e.activation(out=junk[:, 0:64], in_=pj2s[xi][:, 64:128], func=AF.Square, accum_out=ssq[:, 2:3]), reads=[("pj2", xi)], writes=["junk", "ssq"])
            fw.op("act", lambda e: e.activation(out=rs3[:, 0:3], in_=ssq[:, 0:3], func=AF.Ln, scale=1.0 / 64, bias=EPS), reads=["ssq"], writes=["rs3"])
            fw.op("act", lambda e: e.activation(out=rs3[:, 0:3], in_=rs3[:, 0:3], func=AF.Exp, scale=-0.5), reads=["rs3"], writes=["rs3"])
            fw.op("dve", lambda e: e.scalar_tensor_tensor(out=qn32[:], in0=pj1s[xi][:, 384:448], scalar=rs3[:, 0:1], in1=gqk[:, 0:64], op0=ALU.mult, op1=ALU.mult),
                  reads=[("pj1", xi), "rs3", "gqk"], writes=["qn32"])
            fw.op("dve", lambda e: e.scalar_tensor_tensor(out=kn32[:], in0=pj1s[xi][:, 448:512], scalar=rs3[:, 1:2], in1=gqk[:, 64:128], op0=ALU.mult, op1=ALU.mult),
                  reads=[("pj1", xi), "rs3", "gqk"], writes=["kn32"])
            fw.op("dve", lambda e: e.scalar_tensor_tensor(out=cqn[:], in0=pj2s[xi][:, 64:128], scalar=rs3[:, 2:3], in1=gqk[:, 128:192], op0=ALU.mult, op1=ALU.mult),
                  reads=[("pj2", xi), "rs3", "gqk"], writes=["cqn"])
            def chain_a():
                yield
                fw.op("pe", lambda e: e.transpose(out=B3[0:64, 0:128], in_=kn32[:], identity=idf[:]), reads=["kn32", "idf"], writes=["B3"])
                fw.op("dve", lambda e: e.tensor_copy(out=KT[0:64, tt * 128:(tt + 1) * 128], in_=B3[0:64, 0:128]), reads=["B3"], writes=[("KT", tt)])
                fw.op("dve", lambda e: e.reduce_sum(out=ksum[:, tt:tt + 1], in_=B3[0:64, 0:128], axis=AX.X), reads=["B3"], writes=["ksum"])
                if tt % 2 == 1:
                    n = tt // 2
                    fw.op("dve", lambda e: e.tensor_tensor(out=kmT[:, n:n + 1], in0=ksum[:, tt - 1:tt], in1=ksum[:, tt:tt + 1], op=ALU.add), reads=["ksum"], writes=["kmT"])
                    fw.op("dve", lambda e: e.tensor_scalar(out=kmT[:, n:n + 1], in0=kmT[:, n:n + 1], scalar1=1.0 / 256, scalar2=None, op0=ALU.mult), reads=["kmT"], writes=["kmT"])
                fw.op("act", lambda e: e.copy(out=VA[:, tt, 0:64], in_=pj2s[xi][:, 0:64]), reads=[("pj2", xi)], writes=[("VA", tt)])
                yield
                fw.op("act", lambda e: e.copy(out=qs[:, 0:64], in_=qn32[:]), reads=["qn32"], writes=["qs"])
                if own > 0:
                    fw.op("pe", lambda e: e.transpose(out=B3[0:64, 128:256], in_=qn32[:], identity=idf[:]), reads=["qn32", "idf"], writes=["B3"])
                    fw.op("dve", lambda e: e.tensor_copy(out=QT32[:], in_=B3[0:64, 128:256]), reads=["B3"], writes=["QT32"])
                    yield
                    fw.op("pe", lambda e: e.matmul(B3[:, 256:256 + own], lhsT=QT32[:], rhs=kmT[:, 0:own], start=True, stop=True), reads=["QT32", "kmT"], writes=["B3"])
                    fw.op("dve", lambda e: e.tensor_copy(out=gatebuf[:, 0:own], in_=B3[:, 256:256 + own]), reads=["B3"], writes=["gatebuf"])
                    fw.op("dve", lambda e: e.max(out=max8[:], in_=gatebuf[:]), reads=["gatebuf"], writes=["max8"])
                    fw.op("dve", lambda e: e.tensor_scalar(out=selpen[:, 0:own], in0=gatebuf[:, 0:own], scalar1=max8[:, 2:3], scalar2=None, op0=ALU.is_ge),
                          reads=["gatebuf", "max8"], writes=["selpen"])
                    fw.op("dve", lambda e: e.scalar_tensor_tensor(out=qs[:, 64:64 + own], in0=selpen[:, 0:own], scalar=BIG, in1=cbt[:, own * 32:own * 32 + own],
                                                                  op0=ALU.mult, op1=ALU.add), reads=["selpen", "cbt"], writes=["qs"])
                fw.op("dve", lambda e: e.tensor_copy(out=qs[:, 64 + own:96], in_=cbt[:, own * 32 + own:own * 32 + 32]), reads=["cbt"], writes=["qs"])
                yield
                fw.op("pe", lambda e: e.transpose(out=B4h[0:96, 0:128], in_=qs[:], identity=idb[:]), reads=["qs", "idb"], writes=["B2"])
                fw.op("dve", lambda e: e.tensor_copy(out=QT[sb][0:96, a * 128:(a + 1) * 128], in_=B4h[0:96, 0:128]), reads=["B2"], writes=[("QT", sb)])
                yield
                fw.op("pe", lambda e: e.transpose(out=B4h[0:64, 128:256], in_=cqn[:], identity=idb[:]), reads=["cqn", "idb"], writes=["B2"])
                fw.op("dve", lambda e: e.tensor_copy(out=CQT[sb][:, a * 128:(a + 1) * 128], in_=B4h[0:64, 128:256]), reads=["B2"], writes=[("CQT", sb)])
                yield

            def chain_b():
                yield
                fw.op("dve", lambda e: e.tensor_tensor(out=zz[:], in0=pj2s[xi][:, 128:192], in1=bgk[:], op=ALU.add), reads=[("pj2", xi), "bgk"], writes=["zz"])
                fw.op("act", lambda e: e.activation(out=e1[:], in_=zz[:], func=AF.Exp, scale=-1.0), reads=["zz"], writes=["e1"])
                fw.op("act", lambda e: e.activation(out=ll[:], in_=e1[:], func=AF.Ln, bias=1.0), reads=["e1"], writes=["ll"])
                fw.op("pe", lambda e: e.matmul(B4[:, 0:64], lhsT=triu[:], rhs=ll[:], start=True, stop=True), reads=["triu", "ll"], writes=["B4"])
                fw.op("pe", lambda e: e.matmul(B4[:, 64:128], lhsT=ones32[:], rhs=ll[:], start=True, stop=True), reads=["ones32", "ll"], writes=["B4"])
                fw.op("pe", lambda e: e.matmul(B4[0:64, 128:129], lhsT=ll[:], rhs=ones32[:, 0:1], start=True, stop=True), reads=["ones32", "ll"], writes=["B4"])
                yield
                fw.op("act", lambda e: e.activation(out=ecum[:], in_=B4[:, 0:64], func=AF.Exp, scale=-1.0 / 16), reads=["B4"], writes=["ecum"])
                fw.op("act", lambda e: e.activation(out=einv[:], in_=B4[:, 0:64], func=AF.Exp, scale=1.0 / 16), reads=["B4"], writes=["einv"])
                fw.op("act", lambda e: e.activation(out=eL[:], in_=B4[:, 64:128], func=AF.Exp, scale=-1.0 / 16), reads=["B4"], writes=["eL"])
                fw.op("act", lambda e: e.activation(out=decay[:], in_=B4[0:64, 128:129], func=AF.Exp, scale=-1.0 / 16), reads=["B4"], writes=["decay"])
                fw.op("dve", lambda e: e.scalar_tensor_tensor(out=qk2[:, 0:64], in0=pj1s[xi][:, 0:64], scalar=0.125, in1=ecum[:], op0=ALU.mult, op1=ALU.mult),
                      reads=[("pj1", xi), "ecum"], writes=["qk2"])
                fw.op("dve", lambda e: e.tensor_tensor(out=kinv32[:], in0=pj1s[xi][:, 64:128], in1=einv[:], op=ALU.mult), reads=[("pj1", xi), "einv"], writes=["kinv32"])
                fw.op("dve", lambda e: e.tensor_copy(out=qk2[:, 64:128], in_=kinv32[:]), reads=["kinv32"], writes=["qk2"])
                fw.op("dve", lambda e: e.tensor_tensor(out=kend[:], in0=kinv32[:], in1=eL[:], op=ALU.mult), reads=["kinv32", "eL"], writes=["kend"])
                yield
                fw.op("act", lambda e: e.copy(out=vg[:], in_=pj1s[xi][:, 128:256]), reads=[("pj1", xi)], writes=["vg"])
                fw.op("pe", lambda e: e.transpose(out=B4h[0:64, 256:384], in_=qk2[:, 0:64], identity=idb[:]), reads=["qk2", "idb"], writes=["B2"])
                fw.op("pe", lambda e: e.transpose(out=B4h[0:64, 384:512], in_=qk2[:, 64:128], identity=idb[:]), reads=["qk2", "idb"], writes=["B2"])
                fw.op("dve", lambda e: e.tensor_copy(out=qkT[:], in_=B4h[0:64, 256:512]), reads=["B2"], writes=["qkT"])
                yield
                fw.op("pe", lambda e: e.matmul(B3[:, 0:128], lhsT=qkT[:, 128:256], rhs=qkT[:, 0:128], start=True, stop=True), reads=["qkT"], writes=["B3"])
                fw.op("dve", lambda e: e.tensor_tensor(out=ATm[:], in0=B3[:, 0:128], in1=triu[:], op=ALU.mult), reads=["B3", "triu"], writes=["ATm"])
                fw.op("pe", lambda e: e.matmul(B4[:, 256:384], lhsT=vg[:], rhs=ATm[:], start=True, stop=False), reads=["vg", "ATm"], writes=["B4"])
                fw.op("pe", lambda e: e.matmul(B4[:, 256:384], lhsT=Sbf[:], rhs=qkT[:, 0:128], start=False, stop=True), reads=["Sbf", "qkT"], writes=["B4"])
                yield
                fw.op("pe", lambda e: e.matmul(B3[0:64, 256:384], lhsT=kend[:], rhs=vg[:], start=True, stop=True), reads=["kend", "vg"], writes=["B3"])
                fw.op("dve", lambda e: e.scalar_tensor_tensor(out=Sst[:], in0=Sst[:], scalar=decay[:, 0:1], in1=B3[0:64, 256:384], op0=ALU.mult, op1=ALU.add),
                      reads=["Sst", "decay", "B3"], writes=["Sst"])
                fw.op("act", lambda e: e.copy(out=Sbf[:], in_=Sst[:]), reads=["Sst"], writes=["Sbf"])
                yield
                fw.op("act", lambda e: e.activation(out=sq[:], in_=B4[:, 256:384], func=AF.Square), reads=["B4"], writes=["sq"])
                fw.op("pe", lambda e: e.matmul(B3[:, 0:128], lhsT=onesb[:], rhs=sq[:], start=True, stop=True), reads=["onesb", "sq"], writes=["B3"])
                fw.op("act", lambda e: e.activation(out=lnt[:], in_=B3[:, 0:128], func=AF.Ln, scale=1.0 / 128, bias=EPS), reads=["B3"], writes=["lnt"])
                fw.op("act", lambda e: e.activation(out=rstdb[:], in_=lnt[:], func=AF.Exp, scale=-0.5), reads=["lnt"], writes=["rstdb"])
                yield
                fw.op("act", lambda e: e.activation(out=e2[:], in_=pj1s[xi][:, 256:384], func=AF.Exp, scale=-1.0), reads=[("pj1", xi)], writes=["e2"])
                fw.op("dve", lambda e: e.tensor_scalar(out=e2[:], in0=e2[:], scalar1=1.0, scalar2=None, op0=ALU.add), reads=["e2"], writes=["e2"])
                fw.op("dve", lambda e: e.reciprocal(out=e2[:], in_=e2[:]), reads=["e2"], writes=["e2"])
                fw.op("dve", lambda e: e.tensor_tensor(out=sr[:], in0=pj1s[xi][:, 256:384], in1=e2[:], op=ALU.mult), reads=[("pj1", xi), "e2"], writes=["sr"])
                yield
                fw.op("pe", lambda e: e.transpose(out=B4h[:, 512:640], in_=sr[:], identity=idb[:]), reads=["sr", "idb"], writes=["B2"])
                fw.op("dve", lambda e: e.tensor_tensor(out=t1[:], in0=B4[:, 256:384], in1=rstdb[:], op=ALU.mult), reads=["B4", "rstdb"], writes=["t1"])
                fw.op("dve", lambda e: e.scalar_tensor_tensor(out=ogT[sb][:, a * 128:(a + 1) * 128], in0=t1[:], scalar=ggla[:, 0:1], in1=B4h[:, 512:640],
                                                              op0=ALU.mult, op1=ALU.mult), reads=["t1", "ggla", "B2"], writes=[("ogT", sb)])
                yield

            gens_ = [chain_a(), chain_b()] + ([nxt] if nxt is not None else [])
            while gens_:
                for g_ in list(gens_):
                    try:
                        next(g_)
                    except StopIteration:
                        gens_.remove(g_)
                yield

        def attn_block(s):
            sb = s % 2
            q4, s4 = s // 4, s % 4
            nkt = 4 * s + 4

            def qk(kt):
                pb = kt % 2
                diag = kt >= 4 * s
                fw.op("pe", lambda e: e.matmul(PS[pb][:, :], lhsT=KT[0:98, kt * 128:(kt + 1) * 128], rhs=QT[sb][0:98, :], start=True, stop=not diag),
                      reads=["KTc", ("KT", kt), ("QT", sb)], writes=[("PS", pb)])
                if diag:
                    c = kt - 4 * s
                    fw.op("pe", lambda e: e.matmul(PS[pb][:, :], lhsT=idb[:], rhs=cm[:, c * 512:(c + 1) * 512], start=False, stop=True),
                          reads=["idb", "cm"], writes=[("PS", pb)])
            qk(0)
            for kt in range(nkt):
                if kt + 1 < nkt:
                    qk(kt + 1)
                pb, xb_ = kt % 2, kt % 3
                fw.op("act", lambda e, pb=pb, xb_=xb_: e.activation(out=pexp[xb_][:], in_=PS[pb][:, :], func=AF.Exp), reads=[("PS", pb)], writes=[("pexp", xb_)])
                fw.op("pe", lambda e, kt=kt, xb_=xb_: e.matmul(PO[:, :], lhsT=VA[:, kt, :], rhs=pexp[xb_][:], start=(kt == 0), stop=(kt == nkt - 1)),
                      reads=["VAc", ("VA", kt), ("pexp", xb_)], writes=["PO"])
                yield
            fw.op("dve", lambda e: e.reciprocal(out=rden[:], in_=PO[64:128, :]), reads=["PO"], writes=["rden"])
            fw.op("dve", lambda e: e.tensor_tensor(out=omT[sb][:], in0=PO[0:64, :], in1=rden[:], op=ALU.mult), reads=["PO", "rden"], writes=[("omT", sb)])
            yield
            for mt in range(2):
                fw.op("pe", lambda e, mt=mt: e.matmul(PS[mt][:, :], lhsT=MKT[:, mt * 128:(mt + 1) * 128], rhs=CQT[sb][:, :], start=True, stop=True),
                      reads=["MKT", ("CQT", sb)], writes=[("PS", mt)])
            for mt in range(2):
                fw.op("act", lambda e, mt=mt: e.activation(out=pexp[mt][:], in_=PS[mt][:, :], func=AF.Exp), reads=[("PS", mt)], writes=[("pexp", mt)])
                fw.op("pe", lambda e, mt=mt: e.matmul(PO[:, :], lhsT=MVA[:, mt, :], rhs=pexp[mt][:], start=(mt == 0), stop=(mt == 1)),
                      reads=["MVA", ("pexp", mt)], writes=["PO"])
            fw.op("dve", lambda e: e.reciprocal(out=rden[:], in_=PO[64:128, :]), reads=["PO"], writes=["rden"])
            fw.op("dve", lambda e: e.tensor_tensor(out=ocT[sb][:], in0=PO[0:64, :], in1=rden[:], op=ALU.mult), reads=["PO", "rden"], writes=[("ocT", sb)])
            yield
            cs = slice(s4 * 512, (s4 + 1) * 512)
            g = gin.ap()
            fw.dma("pool", lambda e: e.dma_start(out=g[q4, 0:128, cs], in_=ogT[sb][:]), reads=[("ogT", sb)], writes=[("gin", q4, s4, 0)])
            fw.dma("pool", lambda e: e.dma_start(out=g[q4, 128:192, cs], in_=omT[sb][:]), reads=[("omT", sb)], writes=[("gin", q4, s4, 1)])
            fw.dma("pool", lambda e: e.dma_start(out=g[q4, 192:256, cs], in_=ocT[sb][:]), reads=[("ocT", sb)], writes=[("gin", q4, s4, 2)])
            if s4 == 3 and debug != 2:
                rk = [("gin", q4, i, j) for i in range(4) for j in range(3)]
                fw.coll(lambda e: e.collective_compute("AllGather", ALU.bypass, replica_groups=[[0, 1, 2, 3], [4, 5, 6, 7]],
                                                       ins=[g[q4]], outs=[gall.ap()[q4]]), reads=rk, writes=[("gall", q4)])
            yield

        def _chain(gens):
            for g_ in gens:
                yield from g_

        def _interleave(ga, gb):
            a_done, b_done = False, gb is None
            while not (a_done and b_done):
                if not a_done:
                    try:
                        next(ga)
                    except StopIteration:
                        a_done = True
                if not b_done:
                    try:
                        next(gb)
                    except StopIteration:
                        b_done = True

        prev = None
        ntile = 4 * nsb
        if ntile:
            for _ in tile_head(0):
                pass
        for s in range(nsb):
            _interleave(_chain([tile_step(4 * s + a, tile_head(4 * s + a + 1) if 4 * s + a + 1 < ntile else None) for a in range(4)]), prev)
            prev = attn_block(s)
        _interleave(iter(()), prev)
        if debug == 2:
            for s_ in range(nsb):
                q4, s4 = s_ // 4, s_ % 4
                rk = [("gin", q4, s4, j) for j in range(3)]
                fw.dma("pool", lambda e, q4=q4, s4=s4: e.dma_start(out=dbg2[q4, :, s4 * 512:(s4 + 1) * 512], in_=gin.ap()[q4, :, s4 * 512:(s4 + 1) * 512]),
                       reads=rk, writes=[("dbg2", s_)])
        if debug == 1:
            for q4 in range(4):
                fw.dma("pool", lambda e, q4=q4: e.dma_start(out=dbg[q4], in_=gall.ap()[q4]), reads=[("gall", q4)], writes=[("dbg", q4)])
        if fw.limit is not None:
            print("LAST OPS", fw.log[-3:])
        print("stage1 ops", fw.emit())

    if debug in (1, 2):
        return nc
    CAP = 640
    TPG = CAP // 128
    NST = 4 * TPG
    x1d = nc.dram_tensor("x1d", [TOK2, DM], F32)
    iota_d = din("iota_row", [1, CAP])
    sid_d = din("sid", [128, NST])
    goff_d = din("goff", [1, 4])
    es_o = contextlib.ExitStack()
    with es_o:
        def SO(name, shape, dt):
            return es_o.enter_context(nc.sbuf_tensor("s2_" + name, list(shape), dt))
        Wg = SO("Wg", [128, 16, 32], F32)
        slotf = SO("slotf", [128, 16], F32)
        slotrow = SO("slotrow", [128, TOK2], F32)
        Whl = SO("Whl", [128, 16, 4, 16], BF16)
        Wsort = SO("Wsort", [128, NST, 8], F32)
        h2s = SO("h2s", [128, 8, 4 * CAP], BF16)
        R2 = SO("R2", [128, NST, DM], BF16)
        xnb = R2
        ysall = R2

        fb = FW(nc, "b")
        es2 = contextlib.ExitStack()
        with es2:
            def S2(name, shape, dt):
                return es2.enter_context(nc.sbuf_tensor("s2a_" + name, list(shape), dt))

            def P2(name, shape, dt):
                return es2.enter_context(nc.psum_tensor("p2a_" + name, list(shape), dt))
            mixq = S2("mixq", [128, 8, TOK2], BF16)
            wo = S2("wo", [128, 8, DM], BF16)
            wr32 = S2("wr32", [128, 8, 36], F32)
            brb = S2("brb", [128, 36], F32)
            gff = S2("gff", [128, 8], F32)
            idf2 = S2("idf2", [128, 128], F32)
            tri2 = S2("tri2", [128, 128], F32)
            striu = S2("striu", [128, 128], BF16)
            ones2 = S2("ones2", [128, 128], F32)
            ones2b = S2("ones2b", [128, 128], BF16)
            goff = S2("goff", [128, 4], F32)
            gm_all = S2("gm_all", [128, 16, 4], BF16)
            diag = S2("diag", [128, 128], F32)
            tmp4 = S2("tmp4", [128, 4], F32)
            whi = S2("whi", [128, 32], BF16)
            wres = S2("wres", [128, 32], F32)
            xt = [S2("xt%d" % i, [128, DM], F32) for i in range(2)]
            x1t = [S2("x1t%d" % i, [128, DM], F32) for i in range(2)]
            xn2 = S2("xn2", [128, DM], F32)
            junk2 = S2("junk2", [128, DM], BF16)
            h32 = S2("h32", [128, 8, 128], F32)
            ss2 = S2("ss2", [128, 4], F32)
            lgb = S2("lgb", [128, 36], F32)
            gsm = S2("gsm", [128, 8], F32)
            eg = S2("eg", [128, 4], F32)
            gmask = S2("gmask", [128, 4], F32)
            m1 = S2("m1", [128, 4], F32)
            m2 = S2("m2", [128, 4], F32)
            eq1 = S2("eq1", [128, 4, 8], F32)
            eq2 = S2("eq2", [128, 4, 8], F32)
            le2 = S2("le2", [128, 4, 8], F32)
            dd = S2("dd", [128, 4], F32)
            ed = S2("ed", [128, 4], F32)
            w1 = S2("w1", [128, 4], F32)
            w2 = S2("w2", [128, 4], F32)
            gw = S2("gw", [128, 4], F32)
            tA = S2("tA", [128, 4, 8], F32)
            tB = S2("tB", [128, 4, 8], F32)
            PX = [P2("B0", [128, 512], F32), P2("B1", [128, 512], F32)]
            PA = [P2("B2", [128, 512], F32), P2("B3", [128, 512], F32)]
            PL = P2("B4", [128, 512], F32)
            PP = P2("B5", [128, 512], F32)
            PR = P2("B6", [128, 512], F32)

            pid = nc.gpsimd.partition_id()
            if debug == 3:
                gall_in = nc.dram_tensor("gall_in", [4, 1024, 2048], BF16, kind="ExternalInput").ap()
                fb.dma("pool", lambda e: e.dma_start(out=gall.ap(), in_=gall_in), writes=["gall"])
            for r in range(4):
                for hf in range(2):
                    fb.dma("pool", lambda e, r=r, hf=hf: e.dma_start(
                        out=mixq[:, 2 * r + hf, :],
                        in_=gall.ap()[bass.ds(pid % 4, 1), r * 256 + hf * 128:r * 256 + hf * 128 + 128, :].rearrange("a p n -> (a p) n")),
                        reads=["gall"], writes=[("mixq", 2 * r + hf)])
            fb.dma("pool", lambda e: e.dma_start(out=wo[:], in_=w_out_p.rearrange("(c p) n -> p c n", p=128)), writes=["wo"])
            fb.dma("sp", lambda e: e.dma_start(out=wr32[:], in_=w_r.rearrange("(c p) n -> p c n", p=128)), writes=["wr32"])
            fb.dma("sp", lambda e: e.dma_start(out=brb[:], in_=b_r.partition_broadcast(128)), writes=["brb"])
            fb.dma("sp", lambda e: e.dma_start(out=gff[:], in_=g_ffn), writes=["gff"])
            fb.dma("sp", lambda e: e.dma_start(out=idf2[:], in_=ident_d), writes=["idf2"])
            fb.dma("sp", lambda e: e.dma_start(out=tri2[:], in_=triu_d), writes=["tri2"])
            fb.dma("sp", lambda e: e.dma_start(out=goff[:], in_=goff_d.partition_broadcast(128)), writes=["goff"])
            fb.op("dve", lambda e: e.tensor_tensor(out=striu[:], in0=tri2[:], in1=idf2[:], op=ALU.subtract), reads=["tri2", "idf2"], writes=["striu"])
            fb.op("dve", lambda e: e.memset(ones2[:], 1.0), writes=["ones2"])
            fb.op("dve", lambda e: e.memset(ones2b[:], 1.0), writes=["ones2b"])
            mixkeys = [("mixq", i) for i in range(8)]

            def s2_head(tt):
                xi = tt % 2
                tk = ("x1t", xi)
                fb.dma("sp", lambda e: e.dma_start(out=xt[xi][:], in_=xo[tt * 128:(tt + 1) * 128, :]), writes=[("xt", xi)])
                for hf in range(2):
                    for c in range(8):
                        fb.op("pe", lambda e, c=c, hf=hf: e.matmul(PX[hf][:, :], lhsT=mixq[:, c, tt * 128:(tt + 1) * 128], rhs=wo[:, c, hf * 512:(hf + 1) * 512],
                                                                 start=(c == 0), stop=(c == 7)), reads=mixkeys + ["wo"], writes=["B%d" % hf])
                    fb.op("dve", lambda e, hf=hf: e.tensor_tensor(out=x1t[xi][:, hf * 512:(hf + 1) * 512], in0=PX[hf][:, :], in1=xt[xi][:, hf * 512:(hf + 1) * 512], op=ALU.add),
                          reads=["B%d" % hf, ("xt", xi)], writes=[tk])
                yield
                fb.dma("sp", lambda e: e.dma_start(out=x1d.ap()[tt * 128:(tt + 1) * 128, :], in_=x1t[xi][:]), reads=[tk], writes=[("x1d", tt)])
                fb.op("act", lambda e: e.activation(out=junk2[:], in_=x1t[xi][:], func=AF.Square, accum_out=ss2[:, 0:1]), reads=[tk], writes=["junk2", "ss2"])
                fb.op("act", lambda e: e.activation(out=ss2[:, 1:2], in_=ss2[:, 0:1], func=AF.Ln, scale=1.0 / DM, bias=EPS), reads=["ss2"], writes=["ss2b"])
                fb.op("act", lambda e: e.activation(out=ss2[:, 2:3], in_=ss2[:, 1:2], func=AF.Exp, scale=-0.5), reads=["ss2b"], writes=["ss2c"])
                fb.op("dve", lambda e: e.tensor_scalar(out=xn2[:], in0=x1t[xi][:], scalar1=ss2[:, 2:3], scalar2=None, op0=ALU.mult), reads=[tk, "ss2c"], writes=["xn2"])
                yield
                fb.op("act", lambda e: e.copy(out=xnb[:, tt, :], in_=xn2[:]), reads=["xn2"], writes=[("xnb", tt)])
                for c in range(8):
                    fb.op("pe", lambda e, c=c: e.transpose(out=PA[c // 4][:, (c % 4) * 128:(c % 4 + 1) * 128], in_=xn2[:, c * 128:(c + 1) * 128], identity=idf2[:]),
                          reads=["xn2", "idf2"], writes=["B%d" % (2 + c // 4)])
                yield
                for hf in range(2):
                    fb.op("dve", lambda e, hf=hf: e.tensor_tensor(out=h32[:, hf * 4:(hf + 1) * 4, :], in0=PA[hf][:, :].rearrange("p (c t) -> p c t", c=4),
                                                                in1=gff[:, hf * 4:(hf + 1) * 4].unsqueeze(2).to_broadcast([128, 4, 128]), op=ALU.mult),
                          reads=["B%d" % (2 + hf), "gff"], writes=[("h32", hf)])
                for c in range(8):
                    fb.op("pe", lambda e, c=c: e.matmul(PL[:, 0:36], lhsT=h32[:, c, :], rhs=wr32[:, c, :], start=(c == 0), stop=(c == 7)),
                          reads=[("h32", 0), ("h32", 1), "wr32"], writes=["B4"])
                yield

            def s2_tail(tt):
                D = lambda fn, r, w: fb.op("dve", fn, reads=r, writes=w)
                D(lambda e: e.tensor_tensor(out=lgb[:], in0=PL[:, 0:36], in1=brb[:], op=ALU.add), ["B4", "brb"], ["lgb"])
                D(lambda e: e.reduce_max(out=gsm[:, 0:1], in_=lgb[:, 0:4], axis=AX.X), ["lgb"], ["g0"])
                D(lambda e: e.tensor_scalar(out=gsm[:, 1:2], in0=gsm[:, 0:1], scalar1=-1.0, scalar2=None, op0=ALU.mult), ["g0"], ["g1"])
                fb.op("act", lambda e: e.activation(out=eg[:], in_=lgb[:, 0:4], func=AF.Exp, bias=gsm[:, 1:2], scale=1.0, accum_out=gsm[:, 2:3]),
                      reads=["lgb", "g1"], writes=["eg", "g2"])
                D(lambda e: e.reciprocal(out=gsm[:, 3:4], in_=gsm[:, 2:3]), ["g2"], ["g3"])
                D(lambda e: e.tensor_scalar(out=gmask[:], in0=lgb[:, 0:4], scalar1=gsm[:, 0:1], scalar2=None, op0=ALU.is_ge), ["lgb", "g0"], ["gmask"])
                le = lgb[:, 4:36].rearrange("p (g k) -> p g k", g=4)
                bc = lambda t: t[:, :].unsqueeze(2).to_broadcast([128, 4, 8])
                yield
                D(lambda e: e.reduce_max(out=m1[:], in_=le, axis=AX.X), ["lgb"], ["m1"])
                D(lambda e: e.tensor_tensor(out=eq1[:], in0=le, in1=bc(m1), op=ALU.is_ge), ["lgb", "m1"], ["eq1"])
                D(lambda e: e.scalar_tensor_tensor(out=le2[:], in0=eq1[:], scalar=-1e30, in1=le, op0=ALU.mult, op1=ALU.add), ["eq1", "lgb"], ["le2"])
                D(lambda e: e.reduce_max(out=m2[:], in_=le2[:], axis=AX.X), ["le2"], ["m2"])
                D(lambda e: e.tensor_tensor(out=eq2[:], in0=le2[:], in1=bc(m2), op=ALU.is_ge), ["le2", "m2"], ["eq2"])
                yield
                D(lambda e: e.tensor_tensor(out=dd[:], in0=m2[:], in1=m1[:], op=ALU.subtract), ["m1", "m2"], ["dd"])
                fb.op("act", lambda e: e.activation(out=ed[:], in_=dd[:], func=AF.Exp), reads=["dd"], writes=["ed"])
                D(lambda e: e.tensor_scalar(out=w1[:], in0=ed[:], scalar1=1.0, scalar2=None, op0=ALU.add), ["ed"], ["w1"])
                D(lambda e: e.reciprocal(out=w1[:], in_=w1[:]), ["w1"], ["w1"])
                D(lambda e: e.tensor_tensor(out=w2[:], in0=ed[:], in1=w1[:], op=ALU.mult), ["ed", "w1"], ["w2"])
                D(lambda e: e.tensor_scalar(out=gw[:], in0=gmask[:], scalar1=gsm[:, 3:4], scalar2=None, op0=ALU.mult), ["gmask", "g3"], ["gw"])
                D(lambda e: e.tensor_tensor(out=w1[:], in0=w1[:], in1=gw[:], op=ALU.mult), ["w1", "gw"], ["w1"])
                D(lambda e: e.tensor_tensor(out=w2[:], in0=w2[:], in1=gw[:], op=ALU.mult), ["w2", "gw"], ["w2"])
                yield
                D(lambda e: e.tensor_tensor(out=tA[:], in0=eq1[:], in1=bc(w1), op=ALU.mult), ["eq1", "w1"], ["tA"])
                D(lambda e: e.tensor_tensor(out=tB[:], in0=eq2[:], in1=bc(w2), op=ALU.mult), ["eq2", "w2"], ["tB"])
                D(lambda e: e.tensor_tensor(out=Wg[:, tt, :].rearrange("p (g k) -> p g k", g=4), in0=tA[:], in1=tB[:], op=ALU.add), ["tA", "tB"], [("Wg", tt)])
                yield
                fb.op("act", lambda e: e.copy(out=whi[:], in_=Wg[:, tt, :]), reads=[("Wg", tt)], writes=["whi"])
                D(lambda e: e.tensor_tensor(out=wres[:], in0=Wg[:, tt, :], in1=whi[:], op=ALU.subtract), [("Wg", tt), "whi"], ["wres"])
                fb.op("act", lambda e: e.copy(out=Whl[:, tt, :, 0:8], in_=whi[:, :].rearrange("p (g k) -> p g k", g=4)), reads=["whi"], writes=[("Whl", tt, 0)])
                fb.op("act", lambda e: e.copy(out=Whl[:, tt, :, 8:16], in_=wres[:, :].rearrange("p (g k) -> p g k", g=4)), reads=["wres"], writes=[("Whl", tt, 1)])
                yield
                fb.op("act", lambda e: e.copy(out=gm_all[:, tt, :], in_=gmask[:]), reads=["gmask"], writes=[("gm", tt)])
                for tp in range(tt):
                    fb.op("pe", lambda e, tp=tp: e.matmul(PP[:, 0:4], lhsT=ones2b[:], rhs=gm_all[:, tp, :], start=(tp == 0), stop=False),
                          reads=["ones2b", ("gm", tp)], writes=["B5"])
                fb.op("pe", lambda e: e.matmul(PP[:, 0:4], lhsT=striu[:], rhs=gm_all[:, tt, :], start=(tt == 0), stop=True), reads=["striu", ("gm", tt)], writes=["B5"])
                yield
                D(lambda e: e.tensor_tensor(out=tmp4[:], in0=PP[:, 0:4], in1=goff[:], op=ALU.add), ["B5", "goff"], ["tmp4"])
                D(lambda e: e.tensor_tensor(out=tmp4[:], in0=tmp4[:], in1=gmask[:], op=ALU.mult), ["tmp4", "gmask"], ["tmp4"])
                D(lambda e: e.reduce_sum(out=slotf[:, tt:tt + 1], in_=tmp4[:], axis=AX.X), ["tmp4"], [("slotf", tt)])
                yield
                D(lambda e: e.tensor_scalar(out=diag[:], in0=idf2[:], scalar1=slotf[:, tt:tt + 1], scalar2=None, op0=ALU.mult), ["idf2", ("slotf", tt)], ["diag"])
                fb.op("pe", lambda e: e.matmul(PR[:, 0:128], lhsT=ones2[:], rhs=diag[:], start=True, stop=True), reads=["ones2", "diag"], writes=["B6"])
                fb.op("act", lambda e: e.copy(out=slotrow[:, tt * 128:(tt + 1) * 128], in_=PR[:, 0:128]), reads=["B6"], writes=[("slotrow", tt)])
                yield

            for _ in s2_head(0):
                pass
            for tt in range(16):
                gens_ = [s2_tail(tt)] + ([s2_head(tt + 1)] if tt + 1 < 16 else [])
                while gens_:
                    for g_ in list(gens_):
                        try:
                            next(g_)
                        except StopIteration:
                            gens_.remove(g_)
            print("stage2a ops", fb.emit())

        CH = ((0, 384), (384, 256))
        fc1 = FW(nc, "c")
        es3 = contextlib.ExitStack()
        with es3:
            def S3(name, shape, dt):
                return es3.enter_context(nc.sbuf_tensor("s2c_" + name, list(shape), dt))

            def P3(name, shape, dt):
                return es3.enter_context(nc.psum_tensor("p2c_" + name, list(shape), dt))
            Ptb = S3("Ptb", [128, 16, 512], BF16)
            iota = S3("iota", [128, CAP], F32)
            slotrel = S3("slotrel", [128, 4, 16], F32)
            gff2 = S3("gff2", [128, 8], F32)
            wtmp = S3("wtmp", [128, 64], F32)
            PB = [P3("B%d" % i, [128, 512], F32) for i in range(4)]
            PW = P3("B4", [128, 512], F32)
            fc1.dma("sp", lambda e: e.dma_start(out=iota[:], in_=iota_d.partition_broadcast(128)), writes=["iota"])
            fc1.dma("sp", lambda e: e.dma_start(out=gff2[:], in_=g_ffn), writes=["gff2"])
            for g in range(4):
                fc1.op("dve", lambda e, g=g: e.tensor_scalar(out=slotrel[:, g, :], in0=slotf[:], scalar1=-1024.0 * g, scalar2=None, op0=ALU.add), writes=[("slotrel", g)])
            for g in range(4):
                for (off, n) in CH:
                    for tt in range(16):
                        fc1.op("dve", lambda e, g=g, off=off, n=n, tt=tt: e.tensor_scalar(out=Ptb[:, tt, 0:n], in0=iota[:, off:off + n], scalar1=slotrel[:, g, tt:tt + 1],
                                                                                     scalar2=None, op0=ALU.is_equal), reads=["iota", ("slotrel", g)], writes=[("Pt", tt)])
                    for c in range(8):
                        c4 = c % 4
                        for tt in range(16):
                            fc1.op("pe", lambda e, c=c, c4=c4, tt=tt, n=n: e.matmul(PB[c4][:, 0:n], lhsT=xnb[:, tt, c * 128:(c + 1) * 128], rhs=Ptb[:, tt, 0:n],
                                                                                 start=(tt == 0), stop=(tt == 15)), reads=[("Pt", tt)], writes=["B%d" % c4])
                        fc1.op("dve", lambda e, c=c, c4=c4, g=g, off=off, n=n: e.tensor_scalar(out=h2s[:, c, g * CAP + off:g * CAP + off + n], in0=PB[c4][:, 0:n],
                                                                                           scalar1=gff2[:, c:c + 1], scalar2=None, op0=ALU.mult),
                               reads=["B%d" % c4, "gff2"], writes=[("h2s", g, off, c)])
                    nj = n // 128
                    for j in range(nj):
                        for tt in range(16):
                            fc1.op("pe", lambda e, j=j, tt=tt, g=g: e.matmul(PW[:, j * 16:(j + 1) * 16], lhsT=Ptb[:, tt, j * 128:(j + 1) * 128], rhs=Whl[:, tt, g, :],
                                                                           start=(tt == 0), stop=(tt == 15)), reads=[("Pt", tt)], writes=["B4"])
                    st0 = g * TPG + off // 128
                    fc1.op("act", lambda e, nj=nj: e.copy(out=wtmp[:, 0:nj * 16], in_=PW[:, 0:nj * 16]), reads=["B4"], writes=["wtmp"])
                    fc1.op("dve", lambda e, nj=nj, st0=st0: e.tensor_tensor(out=Wsort[:, st0:st0 + nj, :], in0=wtmp[:, 0:nj * 16].rearrange("p (j k) -> p j k", j=nj)[:, :, 0:8],
                                                                         in1=wtmp[:, 0:nj * 16].rearrange("p (j k) -> p j k", j=nj)[:, :, 8:16], op=ALU.add),
                           reads=["wtmp"], writes=[("Wsort", st0)])
            print("stage2c1 ops", fc1.emit())

        fc = FW(nc, "d")
        es4 = contextlib.ExitStack()
        with es4:
            def S4(name, shape, dt):
                return es4.enter_context(nc.sbuf_tensor("s2d_" + name, list(shape), dt))

            def P4(name, shape, dt):
                return es4.enter_context(nc.psum_tensor("p2d_" + name, list(shape), dt))
            wgs = [S4("wg%d" % i, [128, 8, FF], BF16) for i in range(2)]
            wus = [S4("wu%d" % i, [128, 8, FF], BF16) for i in range(2)]
            wds = [S4("wd%d" % i, [128, 4, DM], BF16) for i in range(2)]
            sg = [S4("sg%d" % i, [128, 512], BF16) for i in range(2)]
            hid = [S4("hid%d" % i, [128, 4, 512], BF16) for i in range(2)]
            ys = S4("ys", [128, TPG, DM], F32)
            PG = [P4("B0", [128, 512], F32), P4("B1", [128, 512], F32)]
            PU = [P4("B2", [128, 512], F32), P4("B3", [128, 512], F32)]
            PY = [P4("B4", [128, 512], F32), P4("B5", [128, 512], F32)]
            cnt = 0
            for g in range(4):
                for e8 in range(8):
                    ex = g * 8 + e8
                    wb = ex % 2
                    fc.dma("pool", lambda e, ex=ex, wb=wb: e.dma_start(out=wgs[wb][:], in_=w_gate[ex]), writes=[("wg", wb)])
                    fc.dma("pool", lambda e, ex=ex, wb=wb: e.dma_start(out=wus[wb][:], in_=w_up[ex]), writes=[("wu", wb)])
                    fc.dma("pool", lambda e, ex=ex, wb=wb: e.dma_start(out=wds[wb][:], in_=w_down[ex]), writes=[("wd", wb)])
                    for (off, n) in CH:
                        hb = cnt % 2
                        cnt += 1
                        for f in range(4):
                            gb = f % 2
                            for c in range(8):
                                fc.op("pe", lambda e, c=c, f=f, gb=gb, wb=wb, g=g, off=off, n=n: e.matmul(
                                    PG[gb][:, 0:n], lhsT=wgs[wb][:, c, f * 128:(f + 1) * 128], rhs=h2s[:, c, g * CAP + off:g * CAP + off + n],
                                    start=(c == 0), stop=(c == 7)), reads=[("wg", wb)], writes=["B%d" % gb])
                            for c in range(8):
                                fc.op("pe", lambda e, c=c, f=f, gb=gb, wb=wb, g=g, off=off, n=n: e.matmul(
                                    PU[gb][:, 0:n], lhsT=wus[wb][:, c, f * 128:(f + 1) * 128], rhs=h2s[:, c, g * CAP + off:g * CAP + off + n],
                                    start=(c == 0), stop=(c == 7)), reads=[("wu", wb)], writes=["B%d" % (2 + gb)])
                            fc.op("act", lambda e, gb=gb, n=n: e.activation(out=sg[gb][:, 0:n], in_=PG[gb][:, 0:n], func=AF.Silu), reads=["B%d" % gb], writes=[("sg", gb)])
                            fc.op("dve", lambda e, gb=gb, hb=hb, f=f, n=n: e.tensor_tensor(out=hid[hb][:, f, 0:n], in0=PU[gb][:, 0:n], in1=sg[gb][:, 0:n], op=ALU.mult),
                                  reads=["B%d" % (2 + gb), ("sg", gb)], writes=[("hid", hb, f)])
                        for j in range(n // 128):
                            sl = off // 128 + j
                            for hf in range(2):
                                yb = hf
                                for f in range(4):
                                    fc.op("pe", lambda e, f=f, hb=hb, j=j, hf=hf, wb=wb, yb=yb: e.matmul(
                                        PY[yb][:, :], lhsT=hid[hb][:, f, j * 128:(j + 1) * 128], rhs=wds[wb][:, f, hf * 512:(hf + 1) * 512],
                                        start=(f == 0), stop=(f == 3)),
                                        reads=[("hid", hb, 0), ("hid", hb, 1), ("hid", hb, 2), ("hid", hb, 3), ("wd", wb)], writes=["B%d" % (4 + yb)])
                                if e8 == 0:
                                    fc.op("dve", lambda e, yb=yb, sl=sl, hf=hf, g=g, e8=e8: e.tensor_scalar(
                                        out=ys[:, sl, hf * 512:(hf + 1) * 512], in0=PY[yb][:, :], scalar1=Wsort[:, g * TPG + sl, e8:e8 + 1], scalar2=None, op0=ALU.mult),
                                        reads=["B%d" % (4 + yb)], writes=[("ys", sl, hf)])
                                else:
                                    fc.op("dve", lambda e, yb=yb, sl=sl, hf=hf, g=g, e8=e8: e.scalar_tensor_tensor(
                                        out=ys[:, sl, hf * 512:(hf + 1) * 512], in0=PY[yb][:, :], scalar=Wsort[:, g * TPG + sl, e8:e8 + 1],
                                        in1=ys[:, sl, hf * 512:(hf + 1) * 512], op0=ALU.mult, op1=ALU.add),
                                        reads=["B%d" % (4 + yb)], writes=[("ys", sl, hf)])
                for sl in range(TPG):
                    fc.op("act", lambda e, sl=sl, g=g: e.copy(out=ysall[:, g * TPG + sl, :], in_=ys[:, sl, :]), reads=[("ys", sl, 0), ("ys", sl, 1)], writes=[("ysall", g, sl)])
            print("stage2c2 ops", fc.emit())

        fu = FW(nc, "e")
        es5 = contextlib.ExitStack()
        with es5:
            def S5(name, shape, dt):
                return es5.enter_context(nc.sbuf_tensor("s2e_" + name, list(shape), dt))

            def P5(name, shape, dt):
                return es5.enter_context(nc.psum_tensor("p2e_" + name, list(shape), dt))
            sid = S5("sid", [128, NST], F32)
            PT = [S5("PT%d" % i, [128, NST, 128], BF16) for i in range(2)]
            x1r = [S5("x1r%d" % i, [128, DM], F32) for i in range(2)]
            ot = [S5("ot%d" % i, [128, DM], F32) for i in range(2)]
            PUo = [P5("B%d" % i, [128, 512], F32) for i in range(4)]
            fu.dma("sp", lambda e: e.dma_start(out=sid[:], in_=sid_d), writes=["sid"])
            for tt in range(16):
                xi = tt % 2
                fu.dma("sp", lambda e, tt=tt, xi=xi: e.dma_start(out=x1r[xi][:], in_=x1d.ap()[tt * 128:(tt + 1) * 128, :]), writes=[("x1r", xi)])
                for st in range(NST):
                    eng = "dve"
                    fu.op(eng, lambda e, st=st, tt=tt, xi=xi: e.tensor_scalar(out=PT[xi][:, st, :], in0=slotrow[:, tt * 128:(tt + 1) * 128], scalar1=sid[:, st:st + 1],
                                                                          scalar2=None, op0=ALU.is_equal), reads=["sid"], writes=[("PT", xi, st)])
                for hf in range(2):
                    pb = (tt * 2 + hf) % 4
                    for st in range(NST):
                        fu.op("pe", lambda e, st=st, hf=hf, pb=pb, xi=xi: e.matmul(PUo[pb][:, :], lhsT=PT[xi][:, st, :], rhs=ysall[:, st, hf * 512:(hf + 1) * 512],
                                                                              start=(st == 0), stop=(st == NST - 1)), reads=[("PT", xi, st)], writes=["B%d" % pb])
                    fu.op("dve", lambda e, hf=hf, pb=pb, xi=xi: e.tensor_tensor(out=ot[xi][:, hf * 512:(hf + 1) * 512], in0=PUo[pb][:, :], in1=x1r[xi][:, hf * 512:(hf + 1) * 512], op=ALU.add),
                          reads=["B%d" % pb, ("x1r", xi)], writes=[("ot", xi, hf)])
                fu.dma("sp", lambda e, tt=tt, xi=xi: e.dma_start(out=out[tt * 128:(tt + 1) * 128, :], in_=ot[xi][:]), reads=[("ot", xi, 0), ("ot", xi, 1)], writes=[("out", tt)])
            print("stage2c3 ops", fu.emit())
    return nc


def make_inputs(inputs):
    f = lambda a: np.ascontiguousarray(np.asarray(a, dtype=np.float32))
    x = f(inputs["x"]); mem = f(inputs["mem"])
    w_in = f(inputs["w_in"])[0]
    o_gq, o_gk, o_gv, o_gr, o_lr, o_mq, o_mk, o_mv, o_cq = 0, 256, 512, 1024, 1536, 1552, 1808, 2064, 2320
    gcol = lambda g: f(inputs[g])[0].reshape(8, 128).T.copy()
    w_out = f(inputs["w_out"])[0]
    perm = []
    for h in range(4):
        perm += list(range(128 * h, 128 * h + 128)) + list(range(512 + 64 * h, 512 + 64 * h + 64)) + list(range(768 + 64 * h, 768 + 64 * h + 64))
    w_out_p = np.ascontiguousarray(w_out[perm])
    w_r = np.ascontiguousarray(np.concatenate([f(inputs["w_router_group"])[0]] + [f(inputs["w_router_expert"])[0][g] for g in range(4)], axis=1))
    b_r = np.concatenate([f(inputs["b_router_group"])[0], f(inputs["b_router_expert"])[0].reshape(-1)])[None, :].copy()
    w_gate = np.ascontiguousarray(f(inputs["w_gate"])[0].reshape(NEXP, 8, 128, FF).transpose(0, 2, 1, 3))
    w_up = np.ascontiguousarray(f(inputs["w_up"])[0].reshape(NEXP, 8, 128, FF).transpose(0, 2, 1, 3))
    w_down = np.ascontiguousarray(f(inputs["w_down"])[0].reshape(NEXP, 4, 128, DM).transpose(0, 2, 1, 3))
    ident = np.eye(128, dtype=np.float32)
    triu = np.triu(np.ones((128, 128), np.float32))
    t = np.arange(SEQ)
    maps = []
    for c in range(8):
        b, h = c // 4, c % 4
        m = 2.0 ** (-2.0 * (h + 1))
        cols_a = np.concatenate([np.arange(o_gq + 64 * h, o_gq + 64 * h + 64), np.arange(o_gk + 64 * h, o_gk + 64 * h + 64),
                                 np.arange(o_gv + 128 * h, o_gv + 128 * h + 128), np.arange(o_gr + 128 * h, o_gr + 128 * h + 128),
                                 np.arange(o_mq + 64 * h, o_mq + 64 * h + 64), np.arange(o_mk + 64 * h, o_mk + 64 * h + 64),
                                 np.arange(o_mv + 64 * h, o_mv + 64 * h + 64), np.arange(o_cq + 64 * h, o_cq + 64 * h + 64)])
        kaug = np.zeros((34, SEQ), np.float32)
        kaug[t // 256, t] = 1.0
        kaug[32] = m * (t % 256)
        kaug[33] = 1.0
        qaug = np.zeros((2, 512), np.float32)
        qaug[0] = 1.0
        qaug[1] = -m * (np.arange(512) % 256)
        cb = np.full((32, 32), -BIG, np.float32)
        for own in range(32):
            for n in range(own):
                cb[own, n] = -BIG - m * 256.0 * (own - n)
            cb[own, own] = 0.0
        cmm = np.zeros((4, 128, 512), np.float32)
        for cc in range(4):
            for aa in range(4):
                if cc // 2 != aa // 2:
                    continue
                if aa == cc:
                    blk = np.where(np.arange(128)[:, None] <= np.arange(128)[None, :], 0.0, -BIG)
                elif aa < cc:
                    blk = np.full((128, 128), -BIG)
                else:
                    blk = np.zeros((128, 128))
                cmm[cc, :, aa * 128:(aa + 1) * 128] = blk
        cmm = np.ascontiguousarray(cmm.transpose(1, 0, 2).reshape(128, 2048))
        gq = np.concatenate([f(inputs["moba_q_norm_g"])[0], f(inputs["moba_k_norm_g"])[0], f(inputs["mem_q_norm_g"])[0], f(inputs["mem_k_norm_g"])[0]])[None, :].copy()
        maps.append({
            "xb": x[b], "xo": np.ascontiguousarray(x[b, TOK2 * h:TOK2 * (h + 1)]), "memb": mem[b],
            "w_in_ab": np.ascontiguousarray(w_in[:, cols_a]),
            "w_in_lrT": np.ascontiguousarray(w_in[:, o_lr:o_lr + 16].T),
            "w_gk": np.ascontiguousarray(f(inputs["w_gla_gk"])[0][:, 64 * h:64 * h + 64]),
            "b_gk": np.ascontiguousarray(f(inputs["b_gla_gk"])[0][64 * h:64 * h + 64][None, :]),
            "g_attn": gcol("attn_norm_g"), "g_mem": gcol("mem_norm_g"), "g_ffn": gcol("ffn_norm_g"),
            "g_gla": f(inputs["gla_out_norm_g"])[0][:, None].copy(),
            "g_qk": gq,
            "w_memkv": np.ascontiguousarray(np.concatenate([f(inputs["w_mem_kv"])[0][:, 64 * h:64 * h + 64],
                                                            f(inputs["w_mem_kv"])[0][:, 256 + 64 * h:256 + 64 * h + 64]], axis=1)),
            "w_out_p": w_out_p, "w_r": w_r, "b_r": b_r, "w_gate": w_gate, "w_up": w_up, "w_down": w_down,
            "iota_row": np.arange(640, dtype=np.float32)[None, :].copy(),
            "sid": np.ascontiguousarray((np.arange(128, dtype=np.float32)[:, None] + np.array([(st // 5) * 1024 + (st % 5) * 128 for st in range(20)], np.float32)[None, :])),
            "goff": np.array([[0.0, 1024.0, 2048.0, 3072.0]], np.float32),
            "kaug_c": kaug, "qaug_c": qaug, "cbt": cb.reshape(1, 1024).copy(), "cm": cmm, "ident": ident, "triu": triu,
        })
    return maps


_NC_CACHE = {}


def kernel(**inputs):
    maps = make_inputs(inputs)
    if "nc" not in _NC_CACHE:
        _NC_CACHE["nc"] = build_program(0)
    res = run_bass_kernel_spmd(_NC_CACHE["nc"], maps, core_ids=list(range(8)))
    outp = np.zeros((2, SEQ, DM), np.float32)
    for c in range(8):
        b, h = c // 4, c % 4
        outp[b, TOK2 * h:TOK2 * (h + 1)] = res.results[c]["out"]
    return outp
```
